# Optimizing a Trainium2 kernel written in Bass

```python
import math
import jax, jax.numpy as jnp
from jax import lax
import numpy as np

D_MODEL = 2048
BATCH = 8
SEQ = 2048
DEPTH = 2

N_HEADS = 16
N_KV_HEADS = 4
HEAD_DIM = D_MODEL // N_HEADS
WINDOW = 128
BLOCK = 128
GMLP_GROUPS = 16
GMLP_GROUP_DIM = D_MODEL // GMLP_GROUPS
GMLP_WIDTH = GMLP_GROUPS * GMLP_GROUP_DIM
CHUNK = 128
D_FF_DENSE = 5632
N_EXPERTS = 8
TOP_K = 2
D_FF_EXPERT = 7168
EPS = 1e-6
NEG_INF = -1e30

kernel_name = 'hybrid_swa_gmlp_moe_encoder'


def rmsnorm(x, g):
    xf = x.astype(jnp.float32)
    y = xf * lax.rsqrt(jnp.mean(xf * xf, axis=-1, keepdims=True) + EPS)
    return (y * g.astype(jnp.float32)).astype(x.dtype)


def alibi_slopes(n):
    return jnp.exp2(-8.0 * jnp.arange(1, n + 1, dtype=jnp.float32) / n)


def windowed_gqa_attention(h, w_qkv, q_norm, k_norm, sink, w_o):
    B, S, _ = h.shape
    nb = S // BLOCK
    G = N_HEADS // N_KV_HEADS
    qkv = h @ w_qkv
    q, k, v = jnp.split(qkv, [N_HEADS * HEAD_DIM, (N_HEADS + N_KV_HEADS) * HEAD_DIM], axis=-1)
    q = rmsnorm(q.reshape(B, S, N_HEADS, HEAD_DIM), q_norm)
    k = rmsnorm(k.reshape(B, S, N_KV_HEADS, HEAD_DIM), k_norm)
    v = v.reshape(B, S, N_KV_HEADS, HEAD_DIM)
    q = q.reshape(B, nb, BLOCK, N_KV_HEADS, G, HEAD_DIM)

    def band(t):
        tp = jnp.pad(t, ((0, 0), (BLOCK, BLOCK), (0, 0), (0, 0)))
        tb = tp.reshape(B, nb + 2, BLOCK, N_KV_HEADS, HEAD_DIM)
        return jnp.concatenate([tb[:, :-2], tb[:, 1:-1], tb[:, 2:]], axis=2)

    kb, vb = band(k), band(v)
    scores = jnp.einsum('bnqkgd,bnskd->bkgnqs', q, kb,
                        preferred_element_type=jnp.float32) * (HEAD_DIM ** -0.5)
    qi = jnp.arange(BLOCK)[:, None]
    sj = jnp.arange(3 * BLOCK)[None, :]
    dist = jnp.abs(qi - sj + BLOCK)
    key_pos = jnp.arange(nb)[:, None, None] * BLOCK - BLOCK + sj[None]
    valid = (dist[None] <= WINDOW) & (key_pos >= 0) & (key_pos < S)
    slopes = alibi_slopes(N_HEADS).reshape(N_KV_HEADS, G)
    bias = -slopes[:, :, None, None] * dist.astype(jnp.float32)
    scores = jnp.where(valid, scores + bias[:, :, None], NEG_INF)
    s_logit = sink.astype(jnp.float32).reshape(N_KV_HEADS, G)[None, :, :, None, None, None]
    m = jnp.maximum(jnp.max(scores, axis=-1, keepdims=True), s_logit)
    p = jnp.exp(scores - m)
    probs = p / (jnp.sum(p, axis=-1, keepdims=True) + jnp.exp(s_logit - m))
    out = jnp.einsum('bkgnqs,bnskd->bnqkgd', probs.astype(vb.dtype), vb)
    return out.reshape(B, S, N_HEADS * HEAD_DIM) @ w_o


def chunked_spatial_gating(h, w_in, v_norm, w_s, b_s, w_out):
    B, S, _ = h.shape
    nc = S // CHUNK
    z = jax.nn.gelu(h @ w_in)
    u, v = jnp.split(z, 2, axis=-1)
    v = rmsnorm(v, v_norm).reshape(B, nc, CHUNK, GMLP_GROUPS, GMLP_GROUP_DIM)
    v = jnp.einsum('gts,bcsgd->bctgd', w_s, v) + b_s.T[:, :, None]
    y = u * v.reshape(B, S, GMLP_WIDTH)
    return y @ w_out


def dense_swiglu(h, w_gate_up, w_down):
    g, u = jnp.split(h @ w_gate_up, 2, axis=-1)
    return (jax.nn.silu(g) * u) @ w_down


def moe_swiglu(h, w_router, we_gate, we_up, we_down):
    B, S, D = h.shape
    t = h.reshape(B * S, D)
    logits = (t @ w_router).astype(jnp.float32)
    top_vals, top_idx = lax.top_k(logits, TOP_K)
    top_w = jax.nn.softmax(top_vals, axis=-1)
    gates = jnp.sum(jax.nn.one_hot(top_idx, N_EXPERTS, dtype=jnp.float32) * top_w[..., None], axis=1)
    gates = gates.astype(t.dtype)
    y = jnp.zeros_like(t)
    for e in range(N_EXPERTS):
        he = jax.nn.silu(t @ we_gate[e]) * (t @ we_up[e])
        y = y + gates[:, e:e + 1] * (he @ we_down[e])
    return y.reshape(B, S, D)


def setup_inputs(seed: int = 0) -> dict:
    key = jax.random.key(seed)
    ks = jax.random.split(key, 24)
    f32 = jnp.float32

    def nrm(k, shape, scale):
        return jax.random.normal(k, shape, f32) * scale

    def gain(k, n):
        return 1.0 + 0.02 * jax.random.normal(k, (n,), f32)

    qkv_out = (N_HEADS + 2 * N_KV_HEADS) * HEAD_DIM
    return {
        'x': jax.random.normal(ks[0], (BATCH, SEQ, D_MODEL), f32),
        'l0_mix_norm': gain(ks[1], D_MODEL),
        'l0_w_qkv': nrm(ks[2], (D_MODEL, qkv_out), D_MODEL ** -0.5),
        'l0_q_norm': gain(ks[3], HEAD_DIM),
        'l0_k_norm': gain(ks[4], HEAD_DIM),
        'l0_sink': nrm(ks[5], (N_HEADS,), 1.0),
        'l0_w_o': nrm(ks[6], (N_HEADS * HEAD_DIM, D_MODEL), (N_HEADS * HEAD_DIM) ** -0.5),
        'l0_ffn_norm': gain(ks[7], D_MODEL),
        'l0_w_gate_up': nrm(ks[8], (D_MODEL, 2 * D_FF_DENSE), D_MODEL ** -0.5),
        'l0_w_down': nrm(ks[9], (D_FF_DENSE, D_MODEL), D_FF_DENSE ** -0.5),
        'l1_mix_norm': gain(ks[10], D_MODEL),
        'l1_w_in': nrm(ks[11], (D_MODEL, 2 * GMLP_WIDTH), D_MODEL ** -0.5),
        'l1_v_norm': gain(ks[12], GMLP_WIDTH),
        'l1_w_s': nrm(ks[13], (GMLP_GROUPS, CHUNK, CHUNK), CHUNK ** -0.5),
        'l1_b_s': 1.0 + 0.02 * jax.random.normal(ks[14], (GMLP_GROUPS, CHUNK), f32),
        'l1_w_out': nrm(ks[15], (GMLP_WIDTH, D_MODEL), GMLP_WIDTH ** -0.5),
        'l1_ffn_norm': gain(ks[16], D_MODEL),
        'l1_w_router': nrm(ks[17], (D_MODEL, N_EXPERTS), D_MODEL ** -0.5),
        'l1_we_gate': nrm(ks[18], (N_EXPERTS, D_MODEL, D_FF_EXPERT), D_MODEL ** -0.5),
        'l1_we_up': nrm(ks[19], (N_EXPERTS, D_MODEL, D_FF_EXPERT), D_MODEL ** -0.5),
        'l1_we_down': nrm(ks[20], (N_EXPERTS, D_FF_EXPERT, D_MODEL), D_FF_EXPERT ** -0.5),
    }


def reference(x,
              l0_mix_norm, l0_w_qkv, l0_q_norm, l0_k_norm, l0_sink, l0_w_o,
              l0_ffn_norm, l0_w_gate_up, l0_w_down,
              l1_mix_norm, l1_w_in, l1_v_norm, l1_w_s, l1_b_s, l1_w_out,
              l1_ffn_norm, l1_w_router, l1_we_gate, l1_we_up, l1_we_down):
    layers = [
        dict(mix_norm=l0_mix_norm, mix=(l0_w_qkv, l0_q_norm, l0_k_norm, l0_sink, l0_w_o),
             ffn_norm=l0_ffn_norm, ffn=(l0_w_gate_up, l0_w_down)),
        dict(mix_norm=l1_mix_norm, mix=(l1_w_in, l1_v_norm, l1_w_s, l1_b_s, l1_w_out),
             ffn_norm=l1_ffn_norm, ffn=(l1_w_router, l1_we_gate, l1_we_up, l1_we_down)),
    ]
    for i in range(DEPTH):
        p = layers[i]
        h = rmsnorm(x, p['mix_norm'])
        if i % 2 == 0:
            x = x + windowed_gqa_attention(h, *p['mix'])
        else:
            x = x + chunked_spatial_gating(h, *p['mix'])
        h = rmsnorm(x, p['ffn_norm'])
        if i % 2 == 0:
            x = x + dense_swiglu(h, *p['ffn'])
        else:
            x = x + moe_swiglu(h, *p['ffn'])
    return x
```

```python
import numpy as np
from contextlib import ExitStack
import concourse.bass as bass
import concourse.mybir as mybir
from concourse.bass_utils import run_bass_kernel_spmd

F32 = mybir.dt.float32
BF16 = mybir.dt.bfloat16
AF = mybir.ActivationFunctionType
ALU = mybir.AluOpType
AX = mybir.AxisListType

D = 2048
T = 2048
NT = 16
NCORES = 8
FF0 = 5632
FFE = 7168
NE = 8
EPS = 1e-6
SCALE = 128.0 ** -0.5


class Sched:
    def __init__(self, nc, ndma=20):
        self.nc = nc
        self.eng = {'pe': nc.tensor, 'act': nc.scalar, 'dve': nc.vector, 'pool': nc.gpsimd, 'sp': nc.sync}
        self.sem = {e: nc.alloc_semaphore(f"s_{e}") for e in ('pe', 'act', 'dve', 'pool')}
        self.cnt = {e: 0 for e in self.sem}
        self.seen = {e: {} for e in self.eng}
        self.last_w = {}
        self.readers = {}
        self.dsem = {q: [nc.alloc_semaphore(f"d_{q}_{i}") for i in range(ndma)] for q in ('sp', 'pool')}
        self.dcnt = {q: 0 for q in self.dsem}
        self.dval = {q: [0] * ndma for q in self.dsem}
        self.semkey = {}

    def _wait(self, e, tok):
        sem, val = tok
        if e == 'pe' and sem is self.sem['pe']:
            return
        k = id(sem)
        self.semkey[k] = sem
        if self.seen[e].get(k, 0) < val:
            self.eng[e].wait_ge(sem, val)
            self.seen[e][k] = val

    def _deps(self, e, reads, writes):
        for b in reads:
            w = self.last_w.get(b)
            if w:
                self._wait(e, w)
        for b in writes:
            w = self.last_w.get(b)
            if w:
                self._wait(e, w)
            for r in self.readers.get(b, ()):
                self._wait(e, r)

    def _commit(self, tok, reads, writes):
        for b in reads:
            self.readers.setdefault(b, []).append(tok)
        for b in writes:
            self.last_w[b] = tok
            self.readers[b] = []

    def op(self, e, fn, reads=(), writes=()):
        self._deps(e, reads, writes)
        ins = fn(self.eng[e])
        self.cnt[e] += 1
        ins.then_inc(self.sem[e], 1)
        tok = (self.sem[e], self.cnt[e])
        self._commit(tok, reads, writes)
        return tok

    def dma(self, q, out, in_, reads=(), writes=()):
        self._deps(q, reads, writes)
        ring = self.dsem[q]
        i = self.dcnt[q] % len(ring)
        self.dcnt[q] += 1
        sem = ring[i]
        if self.dval[q][i] > 0:
            self._wait(q, (sem, self.dval[q][i]))
        self.eng[q].dma_start(out=out, in_=in_).then_inc(sem, 16)
        self.dval[q][i] += 16
        tok = (sem, self.dval[q][i])
        self._commit(tok, reads, writes)
        return tok

    def idma(self, out, in_, idxcol, eoff=0, scatter=False, reads=(), writes=()):
        q = 'pool'
        self._deps(q, reads, writes)
        ring = self.dsem[q]
        i = self.dcnt[q] % len(ring)
        self.dcnt[q] += 1
        sem = ring[i]
        if self.dval[q][i] > 0:
            self._wait(q, (sem, self.dval[q][i]))
        off = bass.IndirectOffsetOnAxis(ap=idxcol, axis=0)
        if scatter:
            ins = self.nc.gpsimd.indirect_dma_start(out=out, out_offset=off, in_=in_, in_offset=None,
                                                    element_offset=eoff)
        else:
            ins = self.nc.gpsimd.indirect_dma_start(out=out, out_offset=None, in_=in_, in_offset=off,
                                                    element_offset=eoff)
        ins.then_inc(sem, 16)
        self.dval[q][i] += 16
        tok = (sem, self.dval[q][i])
        self._commit(tok, reads, writes)
        return tok

    def barrier(self):
        toks = [(self.sem[e], self.cnt[e]) for e in self.sem if self.cnt[e] > 0]
        for q in self.dsem:
            for i, s in enumerate(self.dsem[q]):
                if self.dval[q][i] > 0:
                    toks.append((s, self.dval[q][i]))
        for e in self.eng:
            for tok in toks:
                self._wait(e, tok)
        self.last_w = {}
        self.readers = {}


def build_program(stage=99):
    nc = bass.Bass("TRN2", target_bir_lowering=False)

    def din(name, shape):
        return nc.dram_tensor(name, list(shape), F32, kind="ExternalInput").ap()

    x_in = din("x", [T, D])
    g_l0_mix = din("l0_mix_norm", [1, D])
    w_qkv = din("l0_w_qkv", [D, 3072])
    g_q = din("l0_q_norm", [128, 1])
    g_k = din("l0_k_norm", [128, 1])
    sink = din("l0_sink", [1, 16])
    w_o = din("l0_w_o", [D, D])
    g_l0_ffn = din("l0_ffn_norm", [1, D])
    w_gu = din("l0_w_gate_up", [D, 2 * FF0])
    w_dn = din("l0_w_down", [FF0, D])
    g_l1_mix = din("l1_mix_norm", [1, D])
    w_in = din("l1_w_in", [D, 2 * D])
    g_v = din("l1_v_norm", [1, D])
    w_s = din("l1_w_s", [16, 128, 128])
    b_s = din("l1_b_s", [1, 16 * 128])
    w_out = din("l1_w_out", [D, D])
    g_l1_ffn = din("l1_ffn_norm", [1, D])
    if stage >= 4:
        w_r = din("l1_w_router", [D, NE])
        we_g = din("l1_we_gate", [NE, D, FFE])
        we_u = din("l1_we_up", [NE, D, FFE])
        we_d = din("l1_we_down", [NE, FFE, D])
    ident_d = din("c_ident", [128, 128])
    if stage >= 4:
        tri_d = din("c_tri", [128, 128])
        pcol_d = din("c_pcol", [128, 1])
    bias_d = din("c_bias", [128, 3 * 16 * 128])
    y_out = nc.dram_tensor("y", [T, D], F32, kind="ExternalOutput").ap()

    xa = nc.dram_tensor("xa", [T, D], F32).ap()
    xb = nc.dram_tensor("xb", [T, D], F32).ap()
    xc = nc.dram_tensor("xc", [T, D], F32).ap()
    msc = nc.dram_tensor("msc", [16, 128, T], BF16).ap()
    NSS = 15
    hs_d = nc.dram_tensor("hs_d", [NSS * 512, D], BF16).ap()
    outs_d = nc.dram_tensor("outs_d", [NSS * 512, D], BF16).ap()

    S = Sched(nc)
    PS = [nc.alloc_psum_tensor(f"ps{i}", [128, 512], F32).ap() for i in range(8)]
    PSB = [p.bitcast(BF16) for p in PS]

    idb = nc.alloc_sbuf_tensor("idb", [128, 128], BF16).ap()
    onesb = nc.alloc_sbuf_tensor("onesb", [128, 128], BF16).ap()
    ones32 = nc.alloc_sbuf_tensor("ones32", [128, 128], F32).ap()
    S.dma('pool', idb, ident_d, writes=['idb'])
    S.op('dve', lambda e: e.memset(onesb, 1.0), writes=['onesb'])
    S.op('dve', lambda e: e.memset(ones32, 1.0), writes=['ones32'])

    rr = {'n': 0, 'u': 0}

    def sbt(name, shape, dt):
        rr['u'] += 1
        return nc.sbuf_tensor(f"{name}_{rr['u']}", shape, dt)

    def evac_engine():
        rr['n'] += 1
        return 'act' if rr['n'] % 2 == 0 else 'dve'

    def copy_op(e, out, in_, reads, writes):
        if e == 'act':
            S.op('act', lambda en: en.activation(out=out, in_=in_, func=AF.Copy), reads=reads, writes=writes)
        else:
            S.op(e, lambda en: en.tensor_copy(out=out, in_=in_), reads=reads, writes=writes)

    def norm_tile(st, src_rows, gb, b, dst=None, dtag=None):
        xt, ht, junk, ss = st['xt'][b], (st['ht'][b] if dst is None else dst), st['junk'], st['ss'][b]
        S.dma('sp', xt, src_rows, writes=[f'xt{b}'])
        S.op('act', lambda e: e.activation(out=junk, in_=xt, func=AF.Square, accum_out=ss[:, 0:1]),
             reads=[f'xt{b}'], writes=['junk', f'ss{b}'])
        S.op('act', lambda e: e.activation(out=ss[:, 1:2], in_=ss[:, 0:1], func=AF.Sqrt, scale=1.0 / D, bias=EPS),
             reads=[f'ss{b}'], writes=[f'ssb{b}'])
        S.op('dve', lambda e: e.reciprocal(out=ss[:, 2:3], in_=ss[:, 1:2]), reads=[f'ssb{b}'], writes=[f'ssc{b}'])
        S.op('dve', lambda e: e.scalar_tensor_tensor(out=ht, in0=xt, scalar=ss[:, 2:3], in1=gb, op0=ALU.mult,
                                                     op1=ALU.mult),
             reads=[f'xt{b}', f'ssc{b}', 'gb'], writes=[dtag or f'ht{b}'])

    def transpose_tile(st, b, dstT, col0, dtag, banks=(6, 7), src=None, stag=None):
        ht = st['ht'][b] if src is None else src
        for hf in range(2):
            pb = banks[hf]

            def tr(e, hf=hf, pb=pb):
                for c in range(8):
                    ins = e.transpose(out=PSB[pb][:, c * 128:(c + 1) * 128],
                                      in_=ht[:, (hf * 8 + c) * 128:(hf * 8 + c + 1) * 128], identity=idb)
                return ins
            S.op('pe', tr, reads=[stag or f'ht{b}', 'idb'], writes=[f'ps{pb}'])
            copy_op(evac_engine(), dstT[:, hf * 8:(hf + 1) * 8, col0:col0 + 128],
                    PSB[pb].rearrange("p (c t) -> p c t", c=8), reads=[f'ps{pb}'], writes=[dtag])

    def alloc_norm_state(es):
        st = {}
        st['xt'] = [es.enter_context(sbt(f"xt{i}", [128, D], F32)).ap() for i in range(2)]
        st['ht'] = [es.enter_context(sbt(f"ht{i}", [128, D], BF16)).ap() for i in range(2)]
        st['junk'] = es.enter_context(sbt("junk", [128, D], BF16)).ap()
        st['ss'] = [es.enter_context(sbt(f"ss{i}", [128, 4], F32)).ap() for i in range(2)]
        st['gb'] = es.enter_context(sbt("gb", [128, D], F32)).ap()
        return st

    def norm_transpose_all(st, src, gain, HT):
        S.dma('sp', st['gb'], gain.broadcast_to([128, D]), writes=['gb'])
        norm_tile(st, src[0:128, :], st['gb'], 0)
        for t in range(NT):
            if t + 1 < NT:
                norm_tile(st, src[(t + 1) * 128:(t + 2) * 128, :], st['gb'], (t + 1) % 2)
            transpose_tile(st, t % 2, HT, t * 128, f'HT{t // 4}')

    def proj_residual(HT, W, src, dst):
        with ExitStack() as es:
            wb = [es.enter_context(sbt(f"prw{i}", [128, 16, 512], BF16)).ap() for i in range(2)]
            xp = [es.enter_context(sbt(f"prx{i}", [128, 512], F32)).ap() for i in range(3)]
            op_ = [es.enter_context(sbt(f"pro{i}", [128, 512], F32)).ap() for i in range(3)]
            mscv = msc.rearrange("c p t -> p c t")
            for g in range(4):
                S.dma('sp', HT[:, :, g * 512:(g + 1) * 512], mscv[:, :, g * 512:(g + 1) * 512], reads=['msc'],
                      writes=[f'HTm{g}'])
            Wv = W.rearrange("(c p) n -> p c n", p=128)
            k = 0
            for n in range(4):
                wbn = n % 2
                S.dma('pool', wb[wbn], Wv[:, :, n * 512:(n + 1) * 512], writes=[f'prw{wbn}'])
                for t in range(NT):
                    pb = k % 4
                    r3 = k % 3
                    k += 1
                    S.dma('sp', xp[r3], src[t * 128:(t + 1) * 128, n * 512:(n + 1) * 512], writes=[f'prx{r3}'])

                    def mm(e, t=t, wbn=wbn, pb=pb):
                        for c in range(16):
                            ins = e.matmul(PS[pb], lhsT=HT[:, c, t * 128:(t + 1) * 128], rhs=wb[wbn][:, c, :],
                                           start=(c == 0), stop=(c == 15))
                        return ins
                    S.op('pe', mm, reads=[f'HTm{t // 4}', f'prw{wbn}'], writes=[f'ps{pb}'])
                    S.op('dve', lambda e, pb=pb, r3=r3: e.tensor_tensor(out=op_[r3], in0=PS[pb], in1=xp[r3],
                                                                        op=ALU.add),
                         reads=[f'ps{pb}', f'prx{r3}'], writes=[f'pro{r3}'])
                    S.dma('sp', dst[t * 128:(t + 1) * 128, n * 512:(n + 1) * 512], op_[r3], reads=[f'pro{r3}'],
                          writes=['dst'])
            S.barrier()

    def attention_phase(src, zero_fill_target=None):
        with ExitStack() as es:
            HT = es.enter_context(sbt("HT", [128, 16, T], BF16)).ap()
            with ExitStack() as es2:
                st = alloc_norm_state(es2)
                norm_transpose_all(st, src, g_l0_mix, HT)
                S.barrier()
            es3 = ExitStack()
            wq = es3.enter_context(sbt("wq", [128, 16, 512], BF16)).ap()
            wk = es3.enter_context(sbt("wk", [128, 16, 128], BF16)).ap()
            wv = es3.enter_context(sbt("wv", [128, 16, 128], BF16)).ap()
            qT = es3.enter_context(sbt("qT", [128, 16, 4, 128], BF16)).ap()
            kT = es3.enter_context(sbt("kT", [128, T], BF16)).ap()
            Vg = es3.enter_context(sbt("Vg", [128, 16, 128], BF16)).ap()
            aT = es3.enter_context(sbt("aT", [128, 4, T], BF16)).ap()
            biasT = es3.enter_context(sbt("biasT", [128, 3, 16, 128], BF16)).ap()
            sexp = es3.enter_context(sbt("sexp", [128, 16], F32)).ap()
            sinkexp = es3.enter_context(sbt("sinkexp", [128, 16, 128], F32)).ap()
            gq = es3.enter_context(sbt("gq", [128, 1], F32)).ap()
            gk = es3.enter_context(sbt("gk", [128, 1], F32)).ap()
            sqb = [es3.enter_context(sbt(f"sqb{i}", [128, 512], F32)).ap() for i in range(2)]
            stdb = [es3.enter_context(sbt(f"stdb{i}", [128, 512], F32)).ap() for i in range(2)]
            rsb = [es3.enter_context(sbt(f"rsb{i}", [128, 512], F32)).ap() for i in range(2)]
            PT = [es3.enter_context(sbt(f"PT{i}", [128, 512], BF16)).ap() for i in range(6)]
            denb = [es3.enter_context(sbt(f"denb{i}", [128, 512], F32)).ap() for i in range(2)]
            rdb = [es3.enter_context(sbt(f"rdb{i}", [128, 512], F32)).ap() for i in range(2)]

            S.dma('pool', biasT, bias_d.rearrange("p (j h q) -> p j h q", j=3, h=16), writes=['biasT'])
            S.dma('sp', gq, g_q, writes=['gq'])
            S.dma('sp', gk, g_k, writes=['gk'])
            S.dma('sp', sexp, sink.broadcast_to([128, 16]), writes=['sexp'])
            if zero_fill_target is not None:
                ztz = es3.enter_context(sbt("ztz", [128, D], BF16)).ap()
                S.op('pool', lambda e: e.memset(ztz, 0.0), writes=['ztz'])
                for i in range(zero_fill_target.shape[0] // 128):
                    S.dma('sp', zero_fill_target[i * 128:(i + 1) * 128, :], ztz, reads=['ztz'], writes=['hs_zero'])
            S.op('act', lambda e: e.activation(out=sexp, in_=sexp, func=AF.Exp), reads=['sexp'], writes=['sexp'])
            S.op('dve', lambda e: e.tensor_copy(out=sinkexp, in_=sexp.unsqueeze(2).broadcast_to([128, 16, 128])),
                 reads=['sexp'], writes=['sinkexp'])
            Wv_ = w_qkv.rearrange("(c p) n -> p c n", p=128)
            kq = 0
            for kv in range(4):
                S.dma('pool', wq, Wv_[:, :, kv * 512:(kv + 1) * 512], writes=['wq'])
                S.dma('pool', wk, Wv_[:, :, 2048 + kv * 128:2048 + (kv + 1) * 128], writes=['wk'])
                S.dma('pool', wv, Wv_[:, :, 2560 + kv * 128:2560 + (kv + 1) * 128], writes=['wv'])
                for tg in range(4):
                    for hd in range(5):
                        pb = kq % 2
                        pb2 = 2 + kq % 2
                        r2 = kq % 2
                        kq += 1
                        wsrc = wq[:, :, hd * 128:(hd + 1) * 128] if hd < 4 else wk
                        wtag = 'wq' if hd < 4 else 'wk'

                        def mm(e, wsrc=wsrc, pb=pb, tg=tg):
                            for c in range(16):
                                ins = e.matmul(PS[pb], lhsT=wsrc[:, c, :], rhs=HT[:, c, tg * 512:(tg + 1) * 512],
                                               start=(c == 0), stop=(c == 15))
                            return ins
                        S.op('pe', mm, reads=[wtag, f'HT{tg}'], writes=[f'ps{pb}'])
                        S.op('act', lambda e, pb=pb, r2=r2: e.activation(out=sqb[r2], in_=PS[pb], func=AF.Square),
                             reads=[f'ps{pb}'], writes=[f'sqb{r2}'])
                        S.op('pe', lambda e, pb2=pb2, r2=r2: e.matmul(PS[pb2], lhsT=ones32, rhs=sqb[r2], start=True,
                                                                      stop=True),
                             reads=[f'sqb{r2}', 'ones32'], writes=[f'ps{pb2}'])
                        S.op('act', lambda e, pb2=pb2, r2=r2: e.activation(out=stdb[r2], in_=PS[pb2], func=AF.Sqrt,
                                                                           scale=1.0 / 128, bias=EPS),
                             reads=[f'ps{pb2}'], writes=[f'stdb{r2}'])
                        S.op('dve', lambda e, r2=r2: e.reciprocal(out=rsb[r2], in_=stdb[r2]), reads=[f'stdb{r2}'],
                             writes=[f'rsb{r2}'])
                        if hd < 4:
                            S.op('dve', lambda e, pb=pb, r2=r2, hd=hd, tg=tg: e.scalar_tensor_tensor(
                                out=qT[:, tg * 4:(tg + 1) * 4, hd, :], in0=PS[pb].rearrange("p (a b) -> p a b", a=4),
                                scalar=gq[:, 0:1], in1=rsb[r2].rearrange("p (a b) -> p a b", a=4), op0=ALU.mult,
                                op1=ALU.mult), reads=[f'ps{pb}', f'rsb{r2}', 'gq'], writes=[f'qT{tg}'])
                        else:
                            S.op('dve', lambda e, pb=pb, r2=r2, tg=tg: e.scalar_tensor_tensor(
                                out=kT[:, tg * 512:(tg + 1) * 512], in0=PS[pb], scalar=gk[:, 0:1], in1=rsb[r2],
                                op0=ALU.mult, op1=ALU.mult), reads=[f'ps{pb}', f'rsb{r2}', 'gk'], writes=['kT'])
                    pbv = 4 + tg % 2

                    def mmv(e, tg=tg, pbv=pbv):
                        for tt in range(4):
                            t = tg * 4 + tt
                            for c in range(16):
                                ins = e.matmul(PS[pbv][:, tt * 128:(tt + 1) * 128], lhsT=HT[:, c, t * 128:(t + 1) * 128],
                                               rhs=wv[:, c, :], start=(c == 0), stop=(c == 15))
                        return ins
                    S.op('pe', mmv, reads=['wv', f'HT{tg}'], writes=[f'ps{pbv}'])
                    copy_op(evac_engine(), Vg[:, tg * 4:(tg + 1) * 4, :], PS[pbv].rearrange("p (a b) -> p a b", a=4),
                            reads=[f'ps{pbv}'], writes=['Vg'])
                for qb in range(16):
                    kbs = [kb for kb in (qb - 1, qb, qb + 1) if 0 <= kb < 16]
                    pts = []
                    for kb in kbs:
                        j = kb - qb + 1
                        pb = kq % 3
                        r6 = kq % 6
                        kq += 1

                        def mms(e, kb=kb, qb=qb, j=j, pb=pb, kv=kv):
                            e.matmul(PS[pb], lhsT=kT[:, kb * 128:(kb + 1) * 128],
                                     rhs=qT[:, qb, :, :].rearrange("p a b -> p (a b)"), start=True, stop=False)
                            return e.matmul(PS[pb], lhsT=idb,
                                            rhs=biasT[:, j, kv * 4:(kv + 1) * 4, :].rearrange("p a b -> p (a b)"),
                                            start=False, stop=True)
                        S.op('pe', mms, reads=['kT', f'qT{qb // 4}', 'biasT', 'idb'], writes=[f'ps{pb}'])
                        S.op('act', lambda e, pb=pb, r6=r6: e.activation(out=PT[r6], in_=PS[pb], func=AF.Exp,
                                                                         scale=SCALE),
                             reads=[f'ps{pb}'], writes=[f'PT{r6}'])
                        pts.append((kb, r6))
                    b2 = qb % 2
                    ppv, pdn = 3 + b2, 5 + b2

                    def mmpv(e, pts=pts, ppv=ppv, pdn=pdn):
                        for i, (kb, r6) in enumerate(pts):
                            e.matmul(PS[ppv], lhsT=Vg[:, kb, :], rhs=PT[r6], start=(i == 0), stop=(i == len(pts) - 1))
                        for i, (kb, r6) in enumerate(pts):
                            ins = e.matmul(PS[pdn], lhsT=onesb, rhs=PT[r6], start=(i == 0), stop=(i == len(pts) - 1))
                        return ins
                    S.op('pe', mmpv, reads=['Vg', 'onesb'] + [f'PT{r6}' for _, r6 in pts],
                         writes=[f'ps{ppv}', f'ps{pdn}'])
                    S.op('dve', lambda e, pdn=pdn, b2=b2, kv=kv: e.tensor_tensor(
                        out=denb[b2], in0=PS[pdn], in1=sinkexp[:, kv * 4:(kv + 1) * 4, :].rearrange("p a b -> p (a b)"),
                        op=ALU.add), reads=[f'ps{pdn}', 'sinkexp'], writes=[f'denb{b2}'])
                    S.op('dve', lambda e, b2=b2: e.reciprocal(out=rdb[b2], in_=denb[b2]), reads=[f'denb{b2}'],
                         writes=[f'rdb{b2}'])
                    S.op('dve', lambda e, ppv=ppv, b2=b2, qb=qb: e.tensor_tensor(
                        out=aT[:, :, qb * 128:(qb + 1) * 128], in0=PS[ppv].rearrange("p (a b) -> p a b", a=4),
                        in1=rdb[b2].rearrange("p (a b) -> p a b", a=4), op=ALU.mult),
                        reads=[f'ps{ppv}', f'rdb{b2}'], writes=['aT'])
                S.dma('sp', msc[kv * 4:(kv + 1) * 4].rearrange("g p t -> p g t"), aT, reads=['aT'], writes=['msc'])
            S.barrier()
            es3.close()
            proj_residual(HT, w_o, src, xa)

    def ffn_phase(src, dst, gain, moe):
        F = FFE if moe else FF0
        NJ = F // 128
        CP = 7 if moe else 11
        NP = NJ // CP
        with ExitStack() as es:
            st = alloc_norm_state(es)
            hTg = es.enter_context(sbt("hTg", [128, 16, 512], BF16)).ap()
            heT = es.enter_context(sbt("heT", [128, NJ, 512], BF16)).ap()
            gus = [es.enter_context(sbt(f"gus{i}", [128, 16, 256], BF16)).ap() for i in range(4)]
            wdp = [es.enter_context(sbt(f"wdp{i}", [128, CP, 512], BF16)).ap() for i in range(3)]
            sgb = [es.enter_context(sbt(f"sgb{i}", [128, 512], F32)).ap() for i in range(2)]
            if moe:
                acc = [es.enter_context(sbt(f"acc{i}", [128, D], F32)).ap() for i in range(4)]
                wrs = es.enter_context(sbt("wrs", [128, 16, NE], BF16)).ap()
                lg = [es.enter_context(sbt(f"lg{i}", [128, 8], F32)).ap() for i in range(4)]
                mx = [es.enter_context(sbt(f"mx{i}", [128, 8], F32)).ap() for i in range(4)]
                gw = [es.enter_context(sbt(f"gw{i}", [128, 4], F32)).ap() for i in range(4)]
                gt = [es.enter_context(sbt(f"gt{i}", [128, 8], F32)).ap() for i in range(4)]
                gt2 = [es.enter_context(sbt(f"gtb{i}", [128, 8], F32)).ap() for i in range(4)]
                S.dma('pool', wrs, w_r.rearrange("(c p) n -> p c n", p=128), writes=['wrs'])
            else:
                xp = [es.enter_context(sbt(f"fx{i}", [128, 512], F32)).ap() for i in range(3)]
                op_ = [es.enter_context(sbt(f"fo{i}", [128, 512], F32)).ap() for i in range(3)]
            S.dma('sp', st['gb'], gain.broadcast_to([128, D]), writes=['gb'])
            kk = {'gu': 0, 'wd': 0, 'ps': 0, 'x': 0}
            def prep_norm(tg_, tt_):
                t_ = tg_ * 4 + tt_
                norm_tile(st, src[t_ * 128:(t_ + 1) * 128, :], st['gb'], t_ % 2)

            def prep_tr(tg_, tt_):
                transpose_tile(st, (tg_ * 4 + tt_) % 2, hTg, tt_ * 128, 'hTg', banks=(0, 1))

            pipelined = not moe
            for tg in range(4):
                for tt in range(4):
                    t = tg * 4 + tt
                    b = t % 2
                    if not pipelined:
                        prep_norm(tg, tt)
                        prep_tr(tg, tt)
                    elif tg == 0:
                        if tt == 0:
                            prep_norm(0, 0)
                        if tt + 1 < 4:
                            prep_norm(0, tt + 1)
                        prep_tr(0, tt)
                    if moe:
                        S.op('pool', lambda e, tt=tt, b=b: e.tensor_copy(out=acc[tt], in_=st['xt'][b]),
                             reads=[f'xt{b}'], writes=[f'acc{tt}'])

                        def mml(e, tt=tt):
                            for c in range(16):
                                ins = e.matmul(PS[2][:, 0:NE], lhsT=hTg[:, c, tt * 128:(tt + 1) * 128], rhs=wrs[:, c, :],
                                               start=(c == 0), stop=(c == 15))
                            return ins
                        S.op('pe', mml, reads=['hTg', 'wrs'], writes=['ps2'])
                        S.op('dve', lambda e, tt=tt: e.tensor_copy(out=lg[tt], in_=PS[2][:, 0:NE]), reads=['ps2'],
                             writes=[f'lg{tt}'])
                        S.op('dve', lambda e, tt=tt: e.max(out=mx[tt], in_=lg[tt]), reads=[f'lg{tt}'],
                             writes=[f'mx{tt}'])
                        S.op('dve', lambda e, tt=tt: e.tensor_tensor(out=gw[tt][:, 0:1], in0=mx[tt][:, 0:1],
                                                                     in1=mx[tt][:, 1:2], op=ALU.subtract),
                             reads=[f'mx{tt}'], writes=[f'gwa{tt}'])
                        S.op('act', lambda e, tt=tt: e.activation(out=gw[tt][:, 1:2], in_=gw[tt][:, 0:1],
                                                                  func=AF.Sigmoid),
                             reads=[f'gwa{tt}'], writes=[f'gwb{tt}'])
                        S.op('act', lambda e, tt=tt: e.activation(out=gw[tt][:, 2:3], in_=gw[tt][:, 0:1],
                                                                  func=AF.Sigmoid, scale=-1.0),
                             reads=[f'gwa{tt}'], writes=[f'gwc{tt}'])
                        S.op('dve', lambda e, tt=tt: e.tensor_scalar(out=gt[tt], in0=lg[tt], scalar1=mx[tt][:, 0:1],
                                                                     scalar2=gw[tt][:, 1:2], op0=ALU.is_equal,
                                                                     op1=ALU.mult),
                             reads=[f'lg{tt}', f'mx{tt}', f'gwb{tt}'], writes=[f'gt{tt}'])
                        S.op('dve', lambda e, tt=tt: e.tensor_scalar(out=gt2[tt], in0=lg[tt], scalar1=mx[tt][:, 1:2],
                                                                     scalar2=gw[tt][:, 2:3], op0=ALU.is_equal,
                                                                     op1=ALU.mult),
                             reads=[f'lg{tt}', f'mx{tt}', f'gwc{tt}'], writes=[f'gtb{tt}'])
                        S.op('dve', lambda e, tt=tt: e.tensor_tensor(out=gt[tt], in0=gt[tt], in1=gt2[tt], op=ALU.add),
                             reads=[f'gt{tt}', f'gtb{tt}'], writes=[f'gt{tt}'])
                for ex in range(NE if moe else 1):
                    if moe:
                        Wg = we_g[ex].rearrange("(c p) n -> p c n", p=128)
                        Wu = we_u[ex].rearrange("(c p) n -> p c n", p=128)
                        Wd = we_d[ex].rearrange("(c p) n -> p c n", p=128)
                    else:
                        Wg = w_gu[:, 0:F].rearrange("(c p) n -> p c n", p=128)
                        Wu = w_gu[:, F:2 * F].rearrange("(c p) n -> p c n", p=128)
                        Wd = w_dn.rearrange("(c p) n -> p c n", p=128)
                    for jp in range(NJ // 2):
                        sg_, su_ = kk['gu'] % 4, (kk['gu'] + 1) % 4
                        kk['gu'] += 2
                        S.dma('pool', gus[sg_], Wg[:, :, jp * 256:(jp + 1) * 256], writes=[f'gus{sg_}'])
                        S.dma('pool', gus[su_], Wu[:, :, jp * 256:(jp + 1) * 256], writes=[f'gus{su_}'])
                        for jj in range(2):
                            j = jp * 2 + jj
                            pg = kk['ps'] % 2
                            pu = 2 + kk['ps'] % 2
                            kk['ps'] += 1

                            def mmg(e, sg_=sg_, jj=jj, pg=pg):
                                for c in range(16):
                                    ins = e.matmul(PS[pg], lhsT=gus[sg_][:, c, jj * 128:(jj + 1) * 128], rhs=hTg[:, c, :],
                                                   start=(c == 0), stop=(c == 15))
                                return ins

                            def mmu(e, su_=su_, jj=jj, pu=pu):
                                for c in range(16):
                                    ins = e.matmul(PS[pu], lhsT=gus[su_][:, c, jj * 128:(jj + 1) * 128], rhs=hTg[:, c, :],
                                                   start=(c == 0), stop=(c == 15))
                                return ins
                            S.op('pe', mmg, reads=[f'gus{sg_}', 'hTg'], writes=[f'ps{pg}'])
                            S.op('pe', mmu, reads=[f'gus{su_}', 'hTg'], writes=[f'ps{pu}'])
                            S.op('act', lambda e, pg=pg: e.activation(out=sgb[pg], in_=PS[pg], func=AF.Silu),
                                 reads=[f'ps{pg}'], writes=[f'sgb{pg}'])
                            S.op('dve', lambda e, pg=pg, pu=pu, j=j: e.tensor_tensor(out=heT[:, j, :], in0=sgb[pg],
                                                                                     in1=PS[pu], op=ALU.mult),
                                 reads=[f'sgb{pg}', f'ps{pu}'], writes=['heT'])
                    for n in range(4):
                        if pipelined and tg + 1 < 4:
                            prep_norm(tg + 1, n)
                        for p in range(NP):
                            wi = kk['wd'] % 3
                            kk['wd'] += 1
                            S.dma('pool', wdp[wi], Wd[:, p * CP:(p + 1) * CP, n * 512:(n + 1) * 512],
                                  writes=[f'wdp{wi}'])
                            for tt in range(4):
                                def mmd(e, wi=wi, tt=tt, p=p):
                                    for cc in range(CP):
                                        ins = e.matmul(PS[4 + tt], lhsT=heT[:, p * CP + cc, tt * 128:(tt + 1) * 128],
                                                       rhs=wdp[wi][:, cc, :], start=(p == 0 and cc == 0),
                                                       stop=(p == NP - 1 and cc == CP - 1))
                                    return ins
                                S.op('pe', mmd, reads=['heT', f'wdp{wi}'], writes=[f'ps{4 + tt}'])
                        for tt in range(4):
                            t = tg * 4 + tt
                            if moe:
                                S.op('dve', lambda e, tt=tt, n=n, ex=ex: e.scalar_tensor_tensor(
                                    out=acc[tt][:, n * 512:(n + 1) * 512], in0=PS[4 + tt], scalar=gt[tt][:, ex:ex + 1],
                                    in1=acc[tt][:, n * 512:(n + 1) * 512], op0=ALU.mult, op1=ALU.add),
                                    reads=[f'ps{4 + tt}', f'gt{tt}', f'acc{tt}'], writes=[f'acc{tt}'])
                            else:
                                r3 = kk['x'] % 3
                                kk['x'] += 1
                                S.dma('sp', xp[r3], src[t * 128:(t + 1) * 128, n * 512:(n + 1) * 512],
                                      writes=[f'fx{r3}'])
                                S.op('dve', lambda e, tt=tt, r3=r3: e.tensor_tensor(out=op_[r3], in0=PS[4 + tt],
                                                                                    in1=xp[r3], op=ALU.add),
                                     reads=[f'ps{4 + tt}', f'fx{r3}'], writes=[f'fo{r3}'])
                                S.dma('sp', dst[t * 128:(t + 1) * 128, n * 512:(n + 1) * 512], op_[r3],
                                      reads=[f'fo{r3}'], writes=['dst'])
                        if pipelined and tg + 1 < 4:
                            prep_tr(tg + 1, n)
                if moe:
                    for tt in range(4):
                        t = tg * 4 + tt
                        S.dma('sp', dst[t * 128:(t + 1) * 128, :], acc[tt], reads=[f'acc{tt}'], writes=['dst'])
            S.barrier()


    def moe_routed_phase(src, dst, gain):
        I32 = mybir.dt.int32
        WG, NCB = 896, 8
        NJ = FFE // 128
        with ExitStack() as es:
            S1i = es.enter_context(sbt("S1i", [128, 16], I32)).ap()
            S2i = es.enter_context(sbt("S2i", [128, 16], I32)).ap()
            W1 = es.enter_context(sbt("W1", [128, 16], F32)).ap()
            W2 = es.enter_context(sbt("W2", [128, 16], F32)).ap()
            IG = es.enter_context(sbt("IG", [128, 16], I32)).ap()
            ID = es.enter_context(sbt("ID", [128, 16], I32)).ap()
            with ExitStack() as es1:
                st = alloc_norm_state(es1)
                HTok = es1.enter_context(sbt("HTok", [128, 16, D], BF16)).ap()
                hT1 = [es1.enter_context(sbt(f"hT1{i}", [128, 16, 128], BF16)).ap() for i in range(2)]
                wrs = es1.enter_context(sbt("wrs", [128, 16, NE], BF16)).ap()
                trib = es1.enter_context(sbt("trib", [128, 128], BF16)).ap()
                pcol = es1.enter_context(sbt("pcol", [128, 1], F32)).ap()
                lg = es1.enter_context(sbt("lg", [128, 16, 8], F32)).ap()
                mx = es1.enter_context(sbt("mx", [128, 16, 8], F32)).ap()
                eq1 = es1.enter_context(sbt("eq1", [128, 16, 8], F32)).ap()
                eq2 = es1.enter_context(sbt("eq2", [128, 16, 8], F32)).ap()
                Mb = es1.enter_context(sbt("Mb", [128, 16, 8], BF16)).ap()
                CS = es1.enter_context(sbt("CS", [128, 16, 8], F32)).ap()
                TT = es1.enter_context(sbt("TT", [128, 16, 8], F32)).ap()
                OFF = es1.enter_context(sbt("OFF", [128, 16, 8], F32)).ap()
                Gp = es1.enter_context(sbt("Gp", [128, 16, 8], F32)).ap()
                tmp3 = es1.enter_context(sbt("tmp3", [128, 16, 8], F32)).ap()
                sm = es1.enter_context(sbt("sm", [128, 96], F32)).ap()
                S1f = es1.enter_context(sbt("S1f", [128, 16], F32)).ap()
                S2f = es1.enter_context(sbt("S2f", [128, 16], F32)).ap()
                IGf = es1.enter_context(sbt("IGf", [128, 16], F32)).ap()
                IDf = es1.enter_context(sbt("IDf", [128, 16], F32)).ap()
                S.dma('sp', st['gb'], gain.broadcast_to([128, D]), writes=['gb'])
                S.dma('pool', wrs, w_r.rearrange("(c p) n -> p c n", p=128), writes=['wrs'])
                S.dma('pool', trib, tri_d, writes=['trib'])
                S.dma('sp', pcol, pcol_d, writes=['pcol'])
                norm_tile(st, src[0:128, :], st['gb'], 0, dst=HTok[:, 0, :], dtag='HTok0')
                for t in range(NT):
                    b = t % 2
                    if t + 1 < NT:
                        norm_tile(st, src[(t + 1) * 128:(t + 2) * 128, :], st['gb'], (t + 1) % 2, dst=HTok[:, t + 1, :],
                                  dtag=f'HTok{t + 1}')
                    transpose_tile(st, b, hT1[b], 0, f'hT1{b}', banks=(6, 7), src=HTok[:, t, :], stag=f'HTok{t}')
                    pl = 4 + b

                    def mml(e, b=b, pl=pl):
                        for c in range(16):
                            ins = e.matmul(PS[pl][:, 0:NE], lhsT=hT1[b][:, c, :], rhs=wrs[:, c, :], start=(c == 0),
                                           stop=(c == 15))
                        return ins
                    S.op('pe', mml, reads=[f'hT1{b}', 'wrs'], writes=[f'ps{pl}'])
                    S.op('dve', lambda e, t=t, pl=pl: e.tensor_copy(out=lg[:, t, :], in_=PS[pl][:, 0:NE]),
                         reads=[f'ps{pl}'], writes=['lg'])
                    S.op('dve', lambda e, t=t: e.max(out=mx[:, t, :], in_=lg[:, t, :]), reads=['lg'], writes=['mx'])
                S.op('dve', lambda e: e.tensor_tensor(out=sm[:, 0:16], in0=mx[:, :, 0], in1=mx[:, :, 1],
                                                      op=ALU.subtract), reads=['mx'], writes=['sm_d'])
                S.op('act', lambda e: e.activation(out=W1, in_=sm[:, 0:16], func=AF.Sigmoid), reads=['sm_d'],
                     writes=['W1'])
                S.op('act', lambda e: e.activation(out=W2, in_=sm[:, 0:16], func=AF.Sigmoid, scale=-1.0),
                     reads=['sm_d'], writes=['W2'])
                S.op('dve', lambda e: e.tensor_tensor(out=eq1, in0=lg, in1=mx[:, :, 0:1].broadcast_to([128, 16, 8]),
                                                      op=ALU.is_equal), reads=['lg', 'mx'], writes=['eq1'])
                S.op('dve', lambda e: e.tensor_tensor(out=eq2, in0=lg, in1=mx[:, :, 1:2].broadcast_to([128, 16, 8]),
                                                      op=ALU.is_equal), reads=['lg', 'mx'], writes=['eq2'])
                S.op('dve', lambda e: e.tensor_tensor(out=Mb, in0=eq1, in1=eq2, op=ALU.add), reads=['eq1', 'eq2'],
                     writes=['Mb'])
                Mb2 = Mb.rearrange("p t e -> p (t e)")
                S.op('pe', lambda e: e.matmul(PS[0][:, 0:128], lhsT=trib, rhs=Mb2, start=True, stop=True),
                     reads=['Mb', 'trib'], writes=['ps0'])
                S.op('pe', lambda e: e.matmul(PS[1][:, 0:128], lhsT=onesb, rhs=Mb2, start=True, stop=True),
                     reads=['Mb', 'onesb'], writes=['ps1'])
                S.op('dve', lambda e: e.tensor_copy(out=CS.rearrange("p t e -> p (t e)"), in_=PS[0][:, 0:128]),
                     reads=['ps0'], writes=['CS'])
                S.op('dve', lambda e: e.tensor_copy(out=TT.rearrange("p t e -> p (t e)"), in_=PS[1][:, 0:128]),
                     reads=['ps1'], writes=['TT'])
                S.op('dve', lambda e: e.memset(OFF[:, 0, :], 0.0), writes=['OFF'])
                for t in range(1, NT):
                    S.op('dve', lambda e, t=t: e.tensor_tensor(out=OFF[:, t, :], in0=OFF[:, t - 1, :],
                                                               in1=TT[:, t - 1, :], op=ALU.add),
                         reads=['OFF', 'TT'], writes=['OFF'])
                cnt = sm[:, 16:24]
                nsl = sm[:, 24:32]
                cum = sm[:, 32:40]
                bas = sm[:, 40:48]
                t8 = sm[:, 48:56]
                ek = sm[:, 56:72]
                S.op('dve', lambda e: e.tensor_tensor(out=cnt, in0=OFF[:, 15, :], in1=TT[:, 15, :], op=ALU.add),
                     reads=['OFF', 'TT'], writes=['cnt'])
                S.op('dve', lambda e: e.tensor_scalar(out=nsl, in0=cnt, scalar1=0.0, scalar2=None, op0=ALU.is_gt),
                     reads=['cnt'], writes=['nsl'])
                for kk_ in (1, 2, 3):
                    S.op('dve', lambda e, kk_=kk_: e.tensor_scalar(out=t8, in0=cnt, scalar1=512.0 * kk_, scalar2=None,
                                                                   op0=ALU.is_gt), reads=['cnt'], writes=['t8'])
                    S.op('dve', lambda e: e.tensor_tensor(out=nsl, in0=nsl, in1=t8, op=ALU.add), reads=['nsl', 't8'],
                         writes=['nsl'])
                S.op('dve', lambda e: e.tensor_copy(out=cum[:, 0:1], in_=nsl[:, 0:1]), reads=['nsl'], writes=['cum'])
                for ee in range(1, NE):
                    S.op('dve', lambda e, ee=ee: e.tensor_tensor(out=cum[:, ee:ee + 1], in0=cum[:, ee - 1:ee],
                                                                 in1=nsl[:, ee:ee + 1], op=ALU.add),
                         reads=['cum', 'nsl'], writes=['cum'])
                S.op('dve', lambda e: e.tensor_tensor(out=bas, in0=cum, in1=nsl, op=ALU.subtract), reads=['cum', 'nsl'],
                     writes=['bas'])
                S.op('dve', lambda e: e.tensor_scalar(out=bas, in0=bas, scalar1=512.0, scalar2=-1.0, op0=ALU.mult,
                                                      op1=ALU.add), reads=['bas'], writes=['bas'])
                S.op('dve', lambda e: e.tensor_tensor(out=Gp, in0=CS, in1=OFF, op=ALU.add), reads=['CS', 'OFF'],
                     writes=['Gp'])
                S.op('dve', lambda e: e.tensor_tensor(out=Gp, in0=Gp, in1=bas.unsqueeze(1).broadcast_to([128, 16, 8]),
                                                      op=ALU.add), reads=['Gp', 'bas'], writes=['Gp'])
                S.op('dve', lambda e: e.tensor_tensor(out=tmp3, in0=Gp, in1=eq1, op=ALU.mult), reads=['Gp', 'eq1'],
                     writes=['tmp3'])
                S.op('dve', lambda e: e.tensor_reduce(out=S1f, in_=tmp3, axis=AX.X, op=ALU.add), reads=['tmp3'],
                     writes=['S1f'])
                S.op('dve', lambda e: e.tensor_tensor(out=tmp3, in0=Gp, in1=eq2, op=ALU.mult), reads=['Gp', 'eq2'],
                     writes=['tmp3'])
                S.op('dve', lambda e: e.tensor_reduce(out=S2f, in_=tmp3, axis=AX.X, op=ALU.add), reads=['tmp3'],
                     writes=['S2f'])
                S.op('dve', lambda e: e.tensor_copy(out=S1i, in_=S1f), reads=['S1f'], writes=['S1i'])
                S.op('dve', lambda e: e.tensor_copy(out=S2i, in_=S2f), reads=['S2f'], writes=['S2i'])
                for k_ in range(NSS):
                    S.op('dve', lambda e, k_=k_: e.tensor_scalar(out=t8, in0=cum, scalar1=float(k_), scalar2=None,
                                                                 op0=ALU.is_le), reads=['cum'], writes=['t8'])
                    S.op('dve', lambda e, k_=k_: e.tensor_reduce(out=ek[:, k_:k_ + 1], in_=t8, axis=AX.X, op=ALU.add),
                         reads=['t8'], writes=['ek'])
                S.op('dve', lambda e: e.tensor_scalar(out=ek, in0=ek, scalar1=float(NE - 1), scalar2=None, op0=ALU.min),
                     reads=['ek'], writes=['ek'])
                S.op('dve', lambda e: e.tensor_scalar(out=IGf, in0=ek, scalar1=float(D * NCB), scalar2=None,
                                                      op0=ALU.mult), reads=['ek'], writes=['IGf'])
                S.op('dve', lambda e: e.scalar_tensor_tensor(out=IGf, in0=pcol.broadcast_to([128, 16]),
                                                             scalar=float(NCB), in1=IGf, op0=ALU.mult, op1=ALU.add),
                     reads=['IGf', 'pcol'], writes=['IGf'])
                S.op('dve', lambda e: e.tensor_scalar(out=IDf, in0=ek, scalar1=float(FFE * 2), scalar2=None,
                                                      op0=ALU.mult), reads=['ek'], writes=['IDf'])
                S.op('dve', lambda e: e.scalar_tensor_tensor(out=IDf, in0=pcol.broadcast_to([128, 16]), scalar=2.0,
                                                             in1=IDf, op0=ALU.mult, op1=ALU.add),
                     reads=['IDf', 'pcol'], writes=['IDf'])
                S.op('dve', lambda e: e.tensor_copy(out=IG, in_=IGf), reads=['IGf'], writes=['IG'])
                S.op('dve', lambda e: e.tensor_copy(out=ID, in_=IDf), reads=['IDf'], writes=['ID'])
                for t in range(NT):
                    S.idma(hs_d, HTok[:, t, :], S1i[:, t:t + 1], scatter=True, reads=[f'HTok{t}', 'S1i', 'hs_d'],
                           writes=[f'hs_s{t}a'])
                    S.idma(hs_d, HTok[:, t, :], S2i[:, t:t + 1], scatter=True, reads=[f'HTok{t}', 'S2i', 'hs_d'],
                           writes=[f'hs_s{t}b'])
                S.barrier()
            with ExitStack() as es2:
                hTs = es2.enter_context(sbt("hTs", [128, 16, 512], BF16)).ap()
                hst = [es2.enter_context(sbt(f"hst{i}", [128, D], BF16)).ap() for i in range(2)]
                heT = es2.enter_context(sbt("heT", [128, NJ, 512], BF16)).ap()
                slab = [es2.enter_context(sbt(f"slab{i}", [128, 16, WG], BF16)).ap() for i in range(3)]
                wdp = [es2.enter_context(sbt(f"wdq{i}", [128, 1024], BF16)).ap() for i in range(5)]
                sgs = es2.enter_context(sbt("sgs", [128, 7, 512], BF16)).ap()
                ost = [es2.enter_context(sbt(f"ost{i}", [128, 512], BF16)).ap() for i in range(8)]
                Wg2 = we_g.rearrange("e d (cb w) -> (e d cb) w", w=WG)
                Wu2 = we_u.rearrange("e d (cb w) -> (e d cb) w", w=WG)
                Wd2 = we_d.rearrange("e f (nh w) -> (e f nh) w", w=1024)
                loads = []
                for k_ in range(NSS):
                    for cb in range(NCB):
                        for which in (0, 1):
                            loads.append(('s', k_, cb, which))
                    for nh in range(2):
                        for j in range(NJ):
                            loads.append(('d', k_, nh, j))
                pos = {L: i for i, L in enumerate(loads)}
                stt = {'emitted': 0, 'ns': 0, 'nd': 0}
                slot_of = {}

                def emit_load(L):
                    if L[0] == 's':
                        _, k_, cb, which = L
                        r = stt['ns'] % 3
                        stt['ns'] += 1
                        slot_of[L] = r
                        Wsrc = Wg2 if which == 0 else Wu2
                        for c in range(16):
                            S.idma(slab[r][:, c, :], Wsrc, IG[:, k_:k_ + 1], eoff=(c * 128 * NCB + cb) * WG,
                                   reads=['IG'], writes=[f'slab{r}_{c}'])
                    else:
                        _, k_, nh, j = L
                        r = stt['nd'] % 5
                        stt['nd'] += 1
                        slot_of[L] = r
                        S.idma(wdp[r], Wd2, ID[:, k_:k_ + 1], eoff=(j * 128 * 2 + nh) * 1024, reads=['ID'],
                               writes=[f'wdq{r}'])

                ring_cap = {'s': 3, 'd': 5}
                done = {'s': 0, 'd': 0}

                def ensure(L, ahead):
                    upto = min(len(loads), pos[L] + 1 + ahead)
                    while stt['emitted'] < upto:
                        nxt = loads[stt['emitted']]
                        ty = nxt[0]
                        if stt['n' + ty] - done[ty] >= ring_cap[ty]:
                            break
                        emit_load(nxt)
                        stt['emitted'] += 1

                ev = 0

                def load_hst(k_, tt):
                    b = tt % 2
                    S.dma('sp', hst[b], hs_d[(k_ * 4 + tt) * 128:(k_ * 4 + tt + 1) * 128, :], writes=[f'hst{b}'])

                load_hst(0, 0)
                load_hst(0, 1)
                for k_ in range(NSS):
                    for tt in range(4):
                        b = tt % 2
                        if tt >= 2:
                            load_hst(k_, tt)
                        transpose_tile(None, b, hTs, tt * 128, 'hTs', banks=(4, 5), src=hst[b], stag=f'hst{b}')
                    if k_ + 1 < NSS:
                        load_hst(k_ + 1, 0)
                        load_hst(k_ + 1, 1)
                    for cb in range(NCB):
                        for which in (0, 1):
                            L = ('s', k_, cb, which)
                            ensure(L, 2)
                            r = slot_of[L]
                            for jj in range(7):
                                j = cb * 7 + jj
                                pb = (0 if which == 0 else 2) + jj % 2

                                def mm(e, r=r, jj=jj, pb=pb):
                                    for c in range(16):
                                        ins = e.matmul(PS[pb], lhsT=slab[r][:, c, jj * 128:(jj + 1) * 128],
                                                       rhs=hTs[:, c, :], start=(c == 0), stop=(c == 15))
                                    return ins
                                S.op('pe', mm, reads=[f'slab{r}_{c}' for c in range(16)] + ['hTs'], writes=[f'ps{pb}'])
                                if which == 0:
                                    S.op('act', lambda e, pb=pb, jj=jj: e.activation(out=sgs[:, jj, :], in_=PS[pb],
                                                                                     func=AF.Silu),
                                         reads=[f'ps{pb}'], writes=[f'sgs{jj}'])
                                else:
                                    S.op('dve', lambda e, pb=pb, jj=jj, j=j: e.tensor_tensor(
                                        out=heT[:, j, :], in0=sgs[:, jj, :], in1=PS[pb], op=ALU.mult),
                                        reads=[f'sgs{jj}', f'ps{pb}'], writes=['heT'])
                            done['s'] += 1
                    for nh in range(2):
                        for j in range(NJ):
                            L = ('d', k_, nh, j)
                            ensure(L, 4)
                            r = slot_of[L]

                            def mmd(e, r=r, j=j):
                                for tt in range(4):
                                    for n2 in range(2):
                                        ins = e.matmul(PS[tt * 2 + n2], lhsT=heT[:, j, tt * 128:(tt + 1) * 128],
                                                       rhs=wdp[r][:, n2 * 512:(n2 + 1) * 512], start=(j == 0),
                                                       stop=(j == NJ - 1))
                                return ins
                            S.op('pe', mmd, reads=['heT', f'wdq{r}'], writes=[f'ps{i}' for i in range(8)])
                            done['d'] += 1
                        for tt in range(4):
                            for n2 in range(2):
                                o4 = ev % 8
                                ev += 1
                                pb = tt * 2 + n2
                                copy_op(evac_engine(), ost[o4], PS[pb], reads=[f'ps{pb}'], writes=[f'ost{o4}'])
                                r0 = k_ * 512 + tt * 128
                                c0 = nh * 1024 + n2 * 512
                                S.dma('sp', outs_d[r0:r0 + 128, c0:c0 + 512], ost[o4], reads=[f'ost{o4}'],
                                      writes=['outs_d'])
                S.barrier()
            with ExitStack() as es3:
                xt = [es3.enter_context(sbt(f"cx{i}", [128, D], F32)).ap() for i in range(2)]
                g1 = [es3.enter_context(sbt(f"cg1{i}", [128, D], BF16)).ap() for i in range(2)]
                g2 = [es3.enter_context(sbt(f"cg2{i}", [128, D], BF16)).ap() for i in range(2)]
                ac = [es3.enter_context(sbt(f"cac{i}", [128, D], F32)).ap() for i in range(2)]
                for t in range(NT):
                    b = t % 2
                    S.dma('sp', xt[b], src[t * 128:(t + 1) * 128, :], writes=[f'cx{b}'])
                    S.idma(g1[b], outs_d, S1i[:, t:t + 1], reads=['S1i'], writes=[f'cg1{b}'])
                    S.idma(g2[b], outs_d, S2i[:, t:t + 1], reads=['S2i'], writes=[f'cg2{b}'])
                    S.op('dve', lambda e, b=b, t=t: e.scalar_tensor_tensor(out=ac[b], in0=g1[b], scalar=W1[:, t:t + 1],
                                                                           in1=xt[b], op0=ALU.mult, op1=ALU.add),
                         reads=[f'cg1{b}', f'cx{b}', 'W1'], writes=[f'cac{b}'])
                    S.op('dve', lambda e, b=b, t=t: e.scalar_tensor_tensor(out=ac[b], in0=g2[b], scalar=W2[:, t:t + 1],
                                                                           in1=ac[b], op0=ALU.mult, op1=ALU.add),
                         reads=[f'cg2{b}', f'cac{b}', 'W2'], writes=[f'cac{b}'])
                    S.dma('sp', dst[t * 128:(t + 1) * 128, :], ac[b], reads=[f'cac{b}'], writes=['dst'])
                S.barrier()

    def gmlp_phase(src, dst):
        with ExitStack() as es:
            HT = es.enter_context(sbt("HT", [128, 16, T], BF16)).ap()
            with ExitStack() as es2:
                st = alloc_norm_state(es2)
                norm_transpose_all(st, src, g_l1_mix, HT)
                S.barrier()
            es3 = ExitStack()
            vsb = es3.enter_context(sbt("vsb", [128, 16, D], BF16)).ap()
            wb = [es3.enter_context(sbt(f"gw{i}", [128, 16, 512], BF16)).ap() for i in range(2)]
            vgb = es3.enter_context(sbt("vgb", [128, D], F32)).ap()
            wsT = es3.enter_context(sbt("wsT", [128, 16, 128], BF16)).ap()
            bsb = es3.enter_context(sbt("bsb", [128, 16, 128], F32)).ap()
            tmp = [es3.enter_context(sbt(f"gtmp{i}", [128, 512], F32)).ap() for i in range(2)]
            junk = es3.enter_context(sbt("gjunk", [128, 512], BF16)).ap()
            ssv = es3.enter_context(sbt("ssv", [128, 64], F32)).ap()
            ss1 = es3.enter_context(sbt("ss1", [128, 48], F32)).ap()
            es4 = ExitStack()
            wsn = es4.enter_context(sbt("wsn", [128, 16, 128], BF16)).ap()

            S.dma('sp', vgb, g_v.broadcast_to([128, D]), writes=['vgb'])
            S.dma('sp', bsb.rearrange("p g t -> p (g t)"), b_s.broadcast_to([128, 16 * 128]), writes=['bsb'])
            S.dma('pool', wsn, w_s.rearrange("g t s -> t g s"), writes=['wsn'])
            for hf in range(2):
                def trw(e, hf=hf):
                    for c in range(8):
                        ins = e.transpose(out=PSB[hf][:, c * 128:(c + 1) * 128], in_=wsn[:, hf * 8 + c, :], identity=idb)
                    return ins
                S.op('pe', trw, reads=['wsn', 'idb'], writes=[f'ps{hf}'])
                copy_op('dve', wsT[:, hf * 8:(hf + 1) * 8, :], PSB[hf].rearrange("p (c t) -> p c t", c=8),
                        reads=[f'ps{hf}'], writes=['wsT'])
            S.barrier()
            es4.close()
            ugb = [es3.enter_context(sbt(f"ugb{i}", [128, 512], F32)).ap() for i in range(2)]
            vpb = [es3.enter_context(sbt(f"vpb{i}", [128, 512], F32)).ap() for i in range(2)]
            yTj = [es3.enter_context(sbt(f"yTj{i}", [128, T], BF16)).ap() for i in range(2)]
            Wv_ = w_in.rearrange("(c p) n -> p c n", p=128)
            k = 0
            for n in range(4):
                wbn = n % 2
                S.dma('pool', wb[wbn], Wv_[:, :, D + n * 512:D + (n + 1) * 512], writes=[f'gw{wbn}'])
                for t in range(NT):
                    pb = 2 + k % 4
                    r3 = k % 2
                    k += 1

                    def mm(e, t=t, wbn=wbn, pb=pb):
                        for c in range(16):
                            ins = e.matmul(PS[pb], lhsT=HT[:, c, t * 128:(t + 1) * 128], rhs=wb[wbn][:, c, :],
                                           start=(c == 0), stop=(c == 15))
                        return ins
                    S.op('pe', mm, reads=[f'HT{t // 4}', f'gw{wbn}'], writes=[f'ps{pb}'])
                    S.op('act', lambda e, pb=pb, r3=r3: e.activation(out=tmp[r3], in_=PS[pb], func=AF.Gelu_apprx_tanh),
                         reads=[f'ps{pb}'], writes=[f'gtmp{r3}'])
                    S.op('act', lambda e, r3=r3, t=t, n=n: e.activation(out=junk, in_=tmp[r3], func=AF.Square,
                                                                        accum_out=ssv[:, t * 4 + n:t * 4 + n + 1]),
                         reads=[f'gtmp{r3}'], writes=['gjunk', 'ssv'])
                    S.op('dve', lambda e, r3=r3, t=t, n=n: e.tensor_copy(out=vsb[:, t, n * 512:(n + 1) * 512],
                                                                         in_=tmp[r3]),
                         reads=[f'gtmp{r3}'], writes=[f'vsb{t}'])
            S.op('dve', lambda e: e.tensor_reduce(out=ss1[:, 0:16], in_=ssv.rearrange("p (t n) -> p t n", n=4),
                                                  axis=AX.X, op=ALU.add), reads=['ssv'], writes=['ss1a'])
            S.op('act', lambda e: e.activation(out=ss1[:, 16:32], in_=ss1[:, 0:16], func=AF.Sqrt, scale=1.0 / D,
                                               bias=EPS), reads=['ss1a'], writes=['ss1b'])
            S.op('dve', lambda e: e.reciprocal(out=ss1[:, 32:48], in_=ss1[:, 16:32]), reads=['ss1b'], writes=['ss1c'])
            for t in range(NT):
                S.op('dve', lambda e, t=t: e.scalar_tensor_tensor(out=vsb[:, t, :], in0=vsb[:, t, :],
                                                                  scalar=ss1[:, 32 + t:33 + t], in1=vgb, op0=ALU.mult,
                                                                  op1=ALU.mult),
                     reads=[f'vsb{t}', 'ss1c', 'vgb'], writes=[f'vsb{t}'])
            for jp in range(8):
                wbn = jp % 2
                S.dma('pool', wb[wbn][:, :, 0:256], Wv_[:, :, jp * 256:(jp + 1) * 256], writes=[f'gw{wbn}'])
                for jj in range(2):
                    j = jp * 2 + jj
                    yb = j % 2
                    for tg in range(4):
                        pu = k % 2
                        pv = 2 + k % 2
                        k += 1

                        def mmu(e, wbn=wbn, jj=jj, tg=tg, pu=pu):
                            for c in range(16):
                                ins = e.matmul(PS[pu], lhsT=wb[wbn][:, c, jj * 128:(jj + 1) * 128],
                                               rhs=HT[:, c, tg * 512:(tg + 1) * 512], start=(c == 0), stop=(c == 15))
                            return ins
                        S.op('pe', mmu, reads=[f'gw{wbn}', f'HT{tg}'], writes=[f'ps{pu}'])

                        def mmv(e, j=j, tg=tg, pv=pv):
                            for tt in range(4):
                                ins = e.matmul(PS[pv][:, tt * 128:(tt + 1) * 128],
                                               lhsT=vsb[:, tg * 4 + tt, j * 128:(j + 1) * 128], rhs=wsT[:, j, :],
                                               start=True, stop=True)
                            return ins
                        S.op('pe', mmv, reads=[f'vsb{tg * 4 + tt}' for tt in range(4)] + ['wsT'], writes=[f'ps{pv}'])
                        S.op('act', lambda e, pu=pu: e.activation(out=ugb[pu], in_=PS[pu], func=AF.Gelu_apprx_tanh),
                             reads=[f'ps{pu}'], writes=[f'ugb{pu}'])
                        S.op('dve', lambda e, pv=pv, pu=pu, j=j: e.tensor_tensor(
                            out=vpb[pu].rearrange("p (a b) -> p a b", a=4),
                            in0=PS[pv].rearrange("p (a b) -> p a b", a=4),
                            in1=bsb[:, j:j + 1, :].broadcast_to([128, 4, 128]), op=ALU.add),
                            reads=[f'ps{pv}', 'bsb'], writes=[f'vpb{pu}'])
                        S.op('dve', lambda e, pu=pu, yb=yb, tg=tg: e.tensor_tensor(
                            out=yTj[yb][:, tg * 512:(tg + 1) * 512], in0=vpb[pu], in1=ugb[pu], op=ALU.mult),
                            reads=[f'vpb{pu}', f'ugb{pu}'], writes=[f'yTj{yb}'])
                    S.dma('sp', msc[j], yTj[yb], reads=[f'yTj{yb}'], writes=['msc'])
            S.barrier()
            es3.close()
            proj_residual(HT, w_out, src, dst)

    S.barrier()
    if stage >= 1:
        attention_phase(x_in, hs_d if stage >= 4 else None)
    last = xa
    if stage >= 2:
        ffn_phase(xa, xb, g_l0_ffn, moe=False)
        last = xb
    if stage >= 3:
        gmlp_phase(xb, xc)
        last = xc
    if stage >= 4:
        moe_routed_phase(xc, y_out, g_l1_ffn)
    else:
        with ExitStack() as es:
            cp = [es.enter_context(sbt(f"cp{i}", [128, D], F32)).ap() for i in range(2)]
            for t in range(NT):
                S.dma('sp', cp[t % 2], last[t * 128:(t + 1) * 128, :], reads=['dst'], writes=[f'cp{t % 2}'])
                S.dma('sp', y_out[t * 128:(t + 1) * 128, :], cp[t % 2], reads=[f'cp{t % 2}'], writes=['y'])
    S.barrier()
    return nc


def make_consts():
    ident = np.eye(128, dtype=np.float32)
    slopes = 2.0 ** (-8.0 * np.arange(1, 17, dtype=np.float64) / 16)
    s = np.arange(128)[:, None]
    q = np.arange(128)[None, :]
    bias = np.zeros((128, 3, 16, 128), np.float32)
    for j in range(3):
        delta = q - s + (1 - j) * 128
        dist = np.abs(delta)
        valid = dist <= 128
        for h in range(16):
            b = np.where(valid, -slopes[h] * dist, -1e30) / SCALE
            bias[:, j, h, :] = b.astype(np.float32)
    return ident, bias.reshape(128, 3 * 16 * 128)


_NC_CACHE = {}


def kernel(**inputs):
    stage = int(inputs.pop('_stage', 99))
    ncores = int(inputs.pop('_ncores', NCORES))
    if stage not in _NC_CACHE:
        _NC_CACHE[stage] = build_program(stage)
    nc = _NC_CACHE[stage]
    ident, bias = make_consts()
    f = lambda a: np.ascontiguousarray(np.asarray(a, dtype=np.float32))
    shared = {
        'l0_mix_norm': f(inputs['l0_mix_norm']).reshape(1, D),
        'l0_w_qkv': f(inputs['l0_w_qkv']),
        'l0_q_norm': f(inputs['l0_q_norm']).reshape(128, 1),
        'l0_k_norm': f(inputs['l0_k_norm']).reshape(128, 1),
        'l0_sink': f(inputs['l0_sink']).reshape(1, 16),
        'l0_w_o': f(inputs['l0_w_o']),
        'l0_ffn_norm': f(inputs['l0_ffn_norm']).reshape(1, D),
        'l0_w_gate_up': f(inputs['l0_w_gate_up']),
        'l0_w_down': f(inputs['l0_w_down']),
        'l1_mix_norm': f(inputs['l1_mix_norm']).reshape(1, D),
        'l1_w_in': f(inputs['l1_w_in']),
        'l1_v_norm': f(inputs['l1_v_norm']).reshape(1, D),
        'l1_w_s': f(inputs['l1_w_s']),
        'l1_b_s': f(inputs['l1_b_s']).reshape(1, 16 * 128),
        'l1_w_out': f(inputs['l1_w_out']),
        'l1_ffn_norm': f(inputs['l1_ffn_norm']).reshape(1, D),
        'l1_w_router': f(inputs['l1_w_router']) if stage >= 4 else None,
        'l1_we_gate': f(inputs['l1_we_gate']) if stage >= 4 else None,
        'l1_we_up': f(inputs['l1_we_up']) if stage >= 4 else None,
        'l1_we_down': f(inputs['l1_we_down']) if stage >= 4 else None,
        'c_ident': ident,
        'c_bias': bias,
        'c_tri': np.triu(np.ones((128, 128), np.float32)),
        'c_pcol': np.arange(128, dtype=np.float32).reshape(128, 1),
    }
    if stage < 4:
        for kname in ('l1_w_router', 'l1_we_gate', 'l1_we_up', 'l1_we_down', 'c_tri', 'c_pcol'):
            shared.pop(kname)
    x = f(inputs['x'])
    in_maps = []
    for c in range(ncores):
        m = dict(shared)
        m['x'] = x[c]
        in_maps.append(m)
    res = run_bass_kernel_spmd(nc, in_maps, core_ids=list(range(ncores)))
    return np.stack([np.asarray(r['y'], dtype=np.float32) for r in res.results], axis=0)
```

```python
import numpy as np
from contextlib import ExitStack
import concourse.bass as bass
import concourse.mybir as mybir
from concourse.bass_utils import run_bass_kernel_spmd

F32 = mybir.dt.float32
BF16 = mybir.dt.bfloat16
AF = mybir.ActivationFunctionType
ALU = mybir.AluOpType
AX = mybir.AxisListType

D = 2048
T = 2048
NT = 16
NCORES = 8
FF0 = 5632
FFE = 7168
NE = 8
EPS = 1e-6
SCALE = 128.0 ** -0.5


class Sched:
    def __init__(self, nc, ndma=20):
        self.nc = nc
        self.eng = {'pe': nc.tensor, 'act': nc.scalar, 'dve': nc.vector, 'pool': nc.gpsimd, 'sp': nc.sync}
        self.sem = {e: nc.alloc_semaphore(f"s_{e}") for e in ('pe', 'act', 'dve', 'pool')}
        self.cnt = {e: 0 for e in self.sem}
        self.seen = {e: {} for e in self.eng}
        self.last_w = {}
        self.readers = {}
        self.dsem = {q: [nc.alloc_semaphore(f"d_{q}_{i}") for i in range(ndma)] for q in ('sp', 'pool')}
        self.dcnt = {q: 0 for q in self.dsem}
        self.dval = {q: [0] * ndma for q in self.dsem}
        self.semkey = {}

    def _wait(self, e, tok):
        sem, val = tok
        if e == 'pe' and sem is self.sem['pe']:
            return
        k = id(sem)
        self.semkey[k] = sem
        if self.seen[e].get(k, 0) < val:
            self.eng[e].wait_ge(sem, val)
            self.seen[e][k] = val

    def _deps(self, e, reads, writes):
        for b in reads:
            w = self.last_w.get(b)
            if w:
                self._wait(e, w)
        for b in writes:
            w = self.last_w.get(b)
            if w:
                self._wait(e, w)
            for r in self.readers.get(b, ()):
                self._wait(e, r)

    def _commit(self, tok, reads, writes):
        for b in reads:
            self.readers.setdefault(b, []).append(tok)
        for b in writes:
            self.last_w[b] = tok
            self.readers[b] = []

    def op(self, e, fn, reads=(), writes=()):
        self._deps(e, reads, writes)
        ins = fn(self.eng[e])
        self.cnt[e] += 1
        ins.then_inc(self.sem[e], 1)
        tok = (self.sem[e], self.cnt[e])
        self._commit(tok, reads, writes)
        return tok

    def dma(self, q, out, in_, reads=(), writes=()):
        self._deps(q, reads, writes)
        ring = self.dsem[q]
        i = self.dcnt[q] % len(ring)
        self.dcnt[q] += 1
        sem = ring[i]
        if self.dval[q][i] > 0:
            self._wait(q, (sem, self.dval[q][i]))
        self.eng[q].dma_start(out=out, in_=in_).then_inc(sem, 16)
        self.dval[q][i] += 16
        tok = (sem, self.dval[q][i])
        self._commit(tok, reads, writes)
        return tok

    def idma(self, out, in_, idxcol, eoff=0, scatter=False, reads=(), writes=()):
        q = 'pool'
        self._deps(q, reads, writes)
        ring = self.dsem[q]
        i = self.dcnt[q] % len(ring)
        self.dcnt[q] += 1
        sem = ring[i]
        if self.dval[q][i] > 0:
            self._wait(q, (sem, self.dval[q][i]))
        off = bass.IndirectOffsetOnAxis(ap=idxcol, axis=0)
        if scatter:
            ins = self.nc.gpsimd.indirect_dma_start(out=out, out_offset=off, in_=in_, in_offset=None,
                                                    element_offset=eoff)
        else:
            ins = self.nc.gpsimd.indirect_dma_start(out=out, out_offset=None, in_=in_, in_offset=off,
                                                    element_offset=eoff)
        ins.then_inc(sem, 16)
        self.dval[q][i] += 16
        tok = (sem, self.dval[q][i])
        self._commit(tok, reads, writes)
        return tok

    def barrier(self):
        toks = [(self.sem[e], self.cnt[e]) for e in self.sem if self.cnt[e] > 0]
        for q in self.dsem:
            for i, s in enumerate(self.dsem[q]):
                if self.dval[q][i] > 0:
                    toks.append((s, self.dval[q][i]))
        for e in self.eng:
            for tok in toks:
                self._wait(e, tok)
        self.last_w = {}
        self.readers = {}


def build_program(stage=99):
    nc = bass.Bass("TRN2", target_bir_lowering=False)

    def din(name, shape):
        return nc.dram_tensor(name, list(shape), F32, kind="ExternalInput").ap()

    x_in = din("x", [T, D])
    g_l0_mix = din("l0_mix_norm", [1, D])
    w_qkv = din("l0_w_qkv", [D, 3072])
    g_q = din("l0_q_norm", [128, 1])
    g_k = din("l0_k_norm", [128, 1])
    sink = din("l0_sink", [1, 16])
    w_o = din("l0_w_o", [D, D])
    g_l0_ffn = din("l0_ffn_norm", [1, D])
    w_gu = din("l0_w_gate_up", [D, 2 * FF0])
    w_dn = din("l0_w_down", [FF0, D])
    g_l1_mix = din("l1_mix_norm", [1, D])
    w_in = din("l1_w_in", [D, 2 * D])
    g_v = din("l1_v_norm", [1, D])
    w_s = din("l1_w_s", [16, 128, 128])
    b_s = din("l1_b_s", [1, 16 * 128])
    w_out = din("l1_w_out", [D, D])
    g_l1_ffn = din("l1_ffn_norm", [1, D])
    if stage >= 4:
        w_r = din("l1_w_router", [D, NE])
        we_g = din("l1_we_gate", [NE * 8 * 4 * 128, 4 * 896])
        we_u = din("l1_we_up", [NE * 8 * 4 * 128, 4 * 896])
        we_d = din("l1_we_down", [NE, FFE, D])
    ident_d = din("c_ident", [128, 128])
    if stage >= 4:
        tri_d = din("c_tri", [128, 128])
        pcol_d = din("c_pcol", [128, 1])
    bias_d = din("c_bias", [128, 3 * 16 * 128])
    y_out = nc.dram_tensor("y", [T, D], F32, kind="ExternalOutput").ap()

    xa = nc.dram_tensor("xa", [T, D], F32).ap()
    xb = nc.dram_tensor("xb", [T, D], F32).ap()
    xc = nc.dram_tensor("xc", [T, D], F32).ap()
    msc = nc.dram_tensor("msc", [16, 128, T], BF16).ap()
    NSS = 15
    hs_d = nc.dram_tensor("hs_d", [NSS * 512, D], BF16).ap()
    outs_d = nc.dram_tensor("outs_d", [NSS * 512, D], BF16).ap()

    S = Sched(nc)
    PS = [nc.alloc_psum_tensor(f"ps{i}", [128, 512], F32).ap() for i in range(8)]
    PSB = [p.bitcast(BF16) for p in PS]

    idb = nc.alloc_sbuf_tensor("idb", [128, 128], BF16).ap()
    onesb = nc.alloc_sbuf_tensor("onesb", [128, 128], BF16).ap()
    ones32 = nc.alloc_sbuf_tensor("ones32", [128, 128], F32).ap()
    S.dma('pool', idb, ident_d, writes=['idb'])
    S.op('dve', lambda e: e.memset(onesb, 1.0), writes=['onesb'])
    S.op('dve', lambda e: e.memset(ones32, 1.0), writes=['ones32'])

    rr = {'n': 0, 'u': 0}

    def sbt(name, shape, dt):
        rr['u'] += 1
        return nc.sbuf_tensor(f"{name}_{rr['u']}", shape, dt)

    def evac_engine():
        rr['n'] += 1
        return 'act' if rr['n'] % 2 == 0 else 'dve'

    def copy_op(e, out, in_, reads, writes):
        if e == 'act':
            S.op('act', lambda en: en.activation(out=out, in_=in_, func=AF.Copy), reads=reads, writes=writes)
        else:
            S.op(e, lambda en: en.tensor_copy(out=out, in_=in_), reads=reads, writes=writes)

    def norm_tile(st, src_rows, gb, b, dst=None, dtag=None):
        xt, ht, junk, ss = st['xt'][b], (st['ht'][b] if dst is None else dst), st['junk'], st['ss'][b]
        S.dma('sp', xt, src_rows, writes=[f'xt{b}'])
        S.op('act', lambda e: e.activation(out=junk, in_=xt, func=AF.Square, accum_out=ss[:, 0:1]),
             reads=[f'xt{b}'], writes=['junk', f'ss{b}'])
        S.op('act', lambda e: e.activation(out=ss[:, 1:2], in_=ss[:, 0:1], func=AF.Sqrt, scale=1.0 / D, bias=EPS),
             reads=[f'ss{b}'], writes=[f'ssb{b}'])
        S.op('dve', lambda e: e.reciprocal(out=ss[:, 2:3], in_=ss[:, 1:2]), reads=[f'ssb{b}'], writes=[f'ssc{b}'])
        S.op('dve', lambda e: e.scalar_tensor_tensor(out=ht, in0=xt, scalar=ss[:, 2:3], in1=gb, op0=ALU.mult,
                                                     op1=ALU.mult),
             reads=[f'xt{b}', f'ssc{b}', 'gb'], writes=[dtag or f'ht{b}'])

    def transpose_tile(st, b, dstT, col0, dtag, banks=(6, 7), src=None, stag=None):
        ht = st['ht'][b] if src is None else src
        for hf in range(2):
            pb = banks[hf]

            def tr(e, hf=hf, pb=pb):
                for c in range(8):
                    ins = e.transpose(out=PSB[pb][:, c * 128:(c + 1) * 128],
                                      in_=ht[:, (hf * 8 + c) * 128:(hf * 8 + c + 1) * 128], identity=idb)
                return ins
            S.op('pe', tr, reads=[stag or f'ht{b}', 'idb'], writes=[f'ps{pb}'])
            copy_op(evac_engine(), dstT[:, hf * 8:(hf + 1) * 8, col0:col0 + 128],
                    PSB[pb].rearrange("p (c t) -> p c t", c=8), reads=[f'ps{pb}'], writes=[dtag])

    def alloc_norm_state(es):
        st = {}
        st['xt'] = [es.enter_context(sbt(f"xt{i}", [128, D], F32)).ap() for i in range(2)]
        st['ht'] = [es.enter_context(sbt(f"ht{i}", [128, D], BF16)).ap() for i in range(2)]
        st['junk'] = es.enter_context(sbt("junk", [128, D], BF16)).ap()
        st['ss'] = [es.enter_context(sbt(f"ss{i}", [128, 4], F32)).ap() for i in range(2)]
        st['gb'] = es.enter_context(sbt("gb", [128, D], F32)).ap()
        return st

    def norm_transpose_all(st, src, gain, HT):
        S.dma('sp', st['gb'], gain.broadcast_to([128, D]), writes=['gb'])
        norm_tile(st, src[0:128, :], st['gb'], 0)
        for t in range(NT):
            if t + 1 < NT:
                norm_tile(st, src[(t + 1) * 128:(t + 2) * 128, :], st['gb'], (t + 1) % 2)
            transpose_tile(st, t % 2, HT, t * 128, f'HT{t // 4}')

    def proj_residual(HT, W, src, dst):
        with ExitStack() as es:
            wb = [es.enter_context(sbt(f"prw{i}", [128, 16, 512], BF16)).ap() for i in range(2)]
            xp = [es.enter_context(sbt(f"prx{i}", [128, 512], F32)).ap() for i in range(3)]
            op_ = [es.enter_context(sbt(f"pro{i}", [128, 512], F32)).ap() for i in range(3)]
            mscv = msc.rearrange("c p t -> p c t")
            for g in range(4):
                S.dma('sp', HT[:, :, g * 512:(g + 1) * 512], mscv[:, :, g * 512:(g + 1) * 512], reads=['msc'],
                      writes=[f'HTm{g}'])
            Wv = W.rearrange("(c p) n -> p c n", p=128)
            k = 0
            for n in range(4):
                wbn = n % 2
                S.dma('pool', wb[wbn], Wv[:, :, n * 512:(n + 1) * 512], writes=[f'prw{wbn}'])
                for t in range(NT):
                    pb = k % 4
                    r3 = k % 3
                    k += 1
                    S.dma('sp', xp[r3], src[t * 128:(t + 1) * 128, n * 512:(n + 1) * 512], writes=[f'prx{r3}'])

                    def mm(e, t=t, wbn=wbn, pb=pb):
                        for c in range(16):
                            ins = e.matmul(PS[pb], lhsT=HT[:, c, t * 128:(t + 1) * 128], rhs=wb[wbn][:, c, :],
                                           start=(c == 0), stop=(c == 15))
                        return ins
                    S.op('pe', mm, reads=[f'HTm{t // 4}', f'prw{wbn}'], writes=[f'ps{pb}'])
                    S.op('dve', lambda e, pb=pb, r3=r3: e.tensor_tensor(out=op_[r3], in0=PS[pb], in1=xp[r3],
                                                                        op=ALU.add),
                         reads=[f'ps{pb}', f'prx{r3}'], writes=[f'pro{r3}'])
                    S.dma('sp', dst[t * 128:(t + 1) * 128, n * 512:(n + 1) * 512], op_[r3], reads=[f'pro{r3}'],
                          writes=['dst'])
            S.barrier()

    def attention_phase(src, zero_fill_target=None):
        with ExitStack() as es:
            HT = es.enter_context(sbt("HT", [128, 16, T], BF16)).ap()
            with ExitStack() as es2:
                st = alloc_norm_state(es2)
                norm_transpose_all(st, src, g_l0_mix, HT)
                S.barrier()
            es3 = ExitStack()
            wq = es3.enter_context(sbt("wq", [128, 16, 512], BF16)).ap()
            wk = es3.enter_context(sbt("wk", [128, 16, 128], BF16)).ap()
            wv = es3.enter_context(sbt("wv", [128, 16, 128], BF16)).ap()
            qT = es3.enter_context(sbt("qT", [128, 16, 4, 128], BF16)).ap()
            kT = es3.enter_context(sbt("kT", [128, T], BF16)).ap()
            Vg = es3.enter_context(sbt("Vg", [128, 16, 128], BF16)).ap()
            aT = es3.enter_context(sbt("aT", [128, 4, T], BF16)).ap()
            biasT = es3.enter_context(sbt("biasT", [128, 3, 16, 128], BF16)).ap()
            sexp = es3.enter_context(sbt("sexp", [128, 16], F32)).ap()
            sinkexp = es3.enter_context(sbt("sinkexp", [128, 16, 128], F32)).ap()
            gq = es3.enter_context(sbt("gq", [128, 1], F32)).ap()
            gk = es3.enter_context(sbt("gk", [128, 1], F32)).ap()
            sqb = [es3.enter_context(sbt(f"sqb{i}", [128, 512], F32)).ap() for i in range(2)]
            stdb = [es3.enter_context(sbt(f"stdb{i}", [128, 512], F32)).ap() for i in range(2)]
            rsb = [es3.enter_context(sbt(f"rsb{i}", [128, 512], F32)).ap() for i in range(2)]
            PT = [es3.enter_context(sbt(f"PT{i}", [128, 512], BF16)).ap() for i in range(6)]
            denb = [es3.enter_context(sbt(f"denb{i}", [128, 512], F32)).ap() for i in range(2)]
            rdb = [es3.enter_context(sbt(f"rdb{i}", [128, 512], F32)).ap() for i in range(2)]

            S.dma('pool', biasT, bias_d.rearrange("p (j h q) -> p j h q", j=3, h=16), writes=['biasT'])
            S.dma('sp', gq, g_q, writes=['gq'])
            S.dma('sp', gk, g_k, writes=['gk'])
            S.dma('sp', sexp, sink.broadcast_to([128, 16]), writes=['sexp'])
            if zero_fill_target is not None:
                ztz = es3.enter_context(sbt("ztz", [128, D], BF16)).ap()
                S.op('pool', lambda e: e.memset(ztz, 0.0), writes=['ztz'])
                for i in range(zero_fill_target.shape[0] // 128):
                    S.dma('sp', zero_fill_target[i * 128:(i + 1) * 128, :], ztz, reads=['ztz'], writes=['hs_zero'])
            S.op('act', lambda e: e.activation(out=sexp, in_=sexp, func=AF.Exp), reads=['sexp'], writes=['sexp'])
            S.op('dve', lambda e: e.tensor_copy(out=sinkexp, in_=sexp.unsqueeze(2).broadcast_to([128, 16, 128])),
                 reads=['sexp'], writes=['sinkexp'])
            Wv_ = w_qkv.rearrange("(c p) n -> p c n", p=128)
            kq = 0
            for kv in range(4):
                S.dma('pool', wq, Wv_[:, :, kv * 512:(kv + 1) * 512], writes=['wq'])
                S.dma('pool', wk, Wv_[:, :, 2048 + kv * 128:2048 + (kv + 1) * 128], writes=['wk'])
                S.dma('pool', wv, Wv_[:, :, 2560 + kv * 128:2560 + (kv + 1) * 128], writes=['wv'])
                for tg in range(4):
                    for hd in range(5):
                        pb = kq % 2
                        pb2 = 2 + kq % 2
                        r2 = kq % 2
                        kq += 1
                        wsrc = wq[:, :, hd * 128:(hd + 1) * 128] if hd < 4 else wk
                        wtag = 'wq' if hd < 4 else 'wk'

                        def mm(e, wsrc=wsrc, pb=pb, tg=tg):
                            for c in range(16):
                                ins = e.matmul(PS[pb], lhsT=wsrc[:, c, :], rhs=HT[:, c, tg * 512:(tg + 1) * 512],
                                               start=(c == 0), stop=(c == 15))
                            return ins
                        S.op('pe', mm, reads=[wtag, f'HT{tg}'], writes=[f'ps{pb}'])
                        S.op('act', lambda e, pb=pb, r2=r2: e.activation(out=sqb[r2], in_=PS[pb], func=AF.Square),
                             reads=[f'ps{pb}'], writes=[f'sqb{r2}'])
                        S.op('pe', lambda e, pb2=pb2, r2=r2: e.matmul(PS[pb2], lhsT=ones32, rhs=sqb[r2], start=True,
                                                                      stop=True),
                             reads=[f'sqb{r2}', 'ones32'], writes=[f'ps{pb2}'])
                        S.op('act', lambda e, pb2=pb2, r2=r2: e.activation(out=stdb[r2], in_=PS[pb2], func=AF.Sqrt,
                                                                           scale=1.0 / 128, bias=EPS),
                             reads=[f'ps{pb2}'], writes=[f'stdb{r2}'])
                        S.op('dve', lambda e, r2=r2: e.reciprocal(out=rsb[r2], in_=stdb[r2]), reads=[f'stdb{r2}'],
                             writes=[f'rsb{r2}'])
                        if hd < 4:
                            S.op('dve', lambda e, pb=pb, r2=r2, hd=hd, tg=tg: e.scalar_tensor_tensor(
                                out=qT[:, tg * 4:(tg + 1) * 4, hd, :], in0=PS[pb].rearrange("p (a b) -> p a b", a=4),
                                scalar=gq[:, 0:1], in1=rsb[r2].rearrange("p (a b) -> p a b", a=4), op0=ALU.mult,
                                op1=ALU.mult), reads=[f'ps{pb}', f'rsb{r2}', 'gq'], writes=[f'qT{tg}'])
                        else:
                            S.op('dve', lambda e, pb=pb, r2=r2, tg=tg: e.scalar_tensor_tensor(
                                out=kT[:, tg * 512:(tg + 1) * 512], in0=PS[pb], scalar=gk[:, 0:1], in1=rsb[r2],
                                op0=ALU.mult, op1=ALU.mult), reads=[f'ps{pb}', f'rsb{r2}', 'gk'], writes=['kT'])
                    pbv = 4 + tg % 2

                    def mmv(e, tg=tg, pbv=pbv):
                        for tt in range(4):
                            t = tg * 4 + tt
                            for c in range(16):
                                ins = e.matmul(PS[pbv][:, tt * 128:(tt + 1) * 128], lhsT=HT[:, c, t * 128:(t + 1) * 128],
                                               rhs=wv[:, c, :], start=(c == 0), stop=(c == 15))
                        return ins
                    S.op('pe', mmv, reads=['wv', f'HT{tg}'], writes=[f'ps{pbv}'])
                    copy_op(evac_engine(), Vg[:, tg * 4:(tg + 1) * 4, :], PS[pbv].rearrange("p (a b) -> p a b", a=4),
                            reads=[f'ps{pbv}'], writes=['Vg'])
                for qb in range(16):
                    kbs = [kb for kb in (qb - 1, qb, qb + 1) if 0 <= kb < 16]
                    pts = []
                    for kb in kbs:
                        j = kb - qb + 1
                        pb = kq % 3
                        r6 = kq % 6
                        kq += 1

                        def mms(e, kb=kb, qb=qb, j=j, pb=pb, kv=kv):
                            e.matmul(PS[pb], lhsT=kT[:, kb * 128:(kb + 1) * 128],
                                     rhs=qT[:, qb, :, :].rearrange("p a b -> p (a b)"), start=True, stop=False)
                            return e.matmul(PS[pb], lhsT=idb,
                                            rhs=biasT[:, j, kv * 4:(kv + 1) * 4, :].rearrange("p a b -> p (a b)"),
                                            start=False, stop=True)
                        S.op('pe', mms, reads=['kT', f'qT{qb // 4}', 'biasT', 'idb'], writes=[f'ps{pb}'])
                        S.op('act', lambda e, pb=pb, r6=r6: e.activation(out=PT[r6], in_=PS[pb], func=AF.Exp,
                                                                         scale=SCALE),
                             reads=[f'ps{pb}'], writes=[f'PT{r6}'])
                        pts.append((kb, r6))
                    b2 = qb % 2
                    ppv, pdn = 3 + b2, 5 + b2

                    def mmpv(e, pts=pts, ppv=ppv, pdn=pdn):
                        for i, (kb, r6) in enumerate(pts):
                            e.matmul(PS[ppv], lhsT=Vg[:, kb, :], rhs=PT[r6], start=(i == 0), stop=(i == len(pts) - 1))
                        for i, (kb, r6) in enumerate(pts):
                            ins = e.matmul(PS[pdn], lhsT=onesb, rhs=PT[r6], start=(i == 0), stop=(i == len(pts) - 1))
                        return ins
                    S.op('pe', mmpv, reads=['Vg', 'onesb'] + [f'PT{r6}' for _, r6 in pts],
                         writes=[f'ps{ppv}', f'ps{pdn}'])
                    S.op('dve', lambda e, pdn=pdn, b2=b2, kv=kv: e.tensor_tensor(
                        out=denb[b2], in0=PS[pdn], in1=sinkexp[:, kv * 4:(kv + 1) * 4, :].rearrange("p a b -> p (a b)"),
                        op=ALU.add), reads=[f'ps{pdn}', 'sinkexp'], writes=[f'denb{b2}'])
                    S.op('dve', lambda e, b2=b2: e.reciprocal(out=rdb[b2], in_=denb[b2]), reads=[f'denb{b2}'],
                         writes=[f'rdb{b2}'])
                    S.op('dve', lambda e, ppv=ppv, b2=b2, qb=qb: e.tensor_tensor(
                        out=aT[:, :, qb * 128:(qb + 1) * 128], in0=PS[ppv].rearrange("p (a b) -> p a b", a=4),
                        in1=rdb[b2].rearrange("p (a b) -> p a b", a=4), op=ALU.mult),
                        reads=[f'ps{ppv}', f'rdb{b2}'], writes=['aT'])
                S.dma('sp', msc[kv * 4:(kv + 1) * 4].rearrange("g p t -> p g t"), aT, reads=['aT'], writes=['msc'])
            S.barrier()
            es3.close()
            proj_residual(HT, w_o, src, xa)

    def ffn_phase(src, dst, gain, moe):
        F = FFE if moe else FF0
        NJ = F // 128
        CP = 7 if moe else 11
        NP = NJ // CP
        with ExitStack() as es:
            st = alloc_norm_state(es)
            hTg = es.enter_context(sbt("hTg", [128, 16, 512], BF16)).ap()
            heT = es.enter_context(sbt("heT", [128, NJ, 512], BF16)).ap()
            gus = [es.enter_context(sbt(f"gus{i}", [128, 16, 256], BF16)).ap() for i in range(4)]
            wdp = [es.enter_context(sbt(f"wdp{i}", [128, CP, 512], BF16)).ap() for i in range(3)]
            sgb = [es.enter_context(sbt(f"sgb{i}", [128, 512], F32)).ap() for i in range(2)]
            if moe:
                acc = [es.enter_context(sbt(f"acc{i}", [128, D], F32)).ap() for i in range(4)]
                wrs = es.enter_context(sbt("wrs", [128, 16, NE], BF16)).ap()
                lg = [es.enter_context(sbt(f"lg{i}", [128, 8], F32)).ap() for i in range(4)]
                mx = [es.enter_context(sbt(f"mx{i}", [128, 8], F32)).ap() for i in range(4)]
                gw = [es.enter_context(sbt(f"gw{i}", [128, 4], F32)).ap() for i in range(4)]
                gt = [es.enter_context(sbt(f"gt{i}", [128, 8], F32)).ap() for i in range(4)]
                gt2 = [es.enter_context(sbt(f"gtb{i}", [128, 8], F32)).ap() for i in range(4)]
                S.dma('pool', wrs, w_r.rearrange("(c p) n -> p c n", p=128), writes=['wrs'])
            else:
                xp = [es.enter_context(sbt(f"fx{i}", [128, 512], F32)).ap() for i in range(3)]
                op_ = [es.enter_context(sbt(f"fo{i}", [128, 512], F32)).ap() for i in range(3)]
            S.dma('sp', st['gb'], gain.broadcast_to([128, D]), writes=['gb'])
            kk = {'gu': 0, 'wd': 0, 'ps': 0, 'x': 0}
            def prep_norm(tg_, tt_):
                t_ = tg_ * 4 + tt_
                norm_tile(st, src[t_ * 128:(t_ + 1) * 128, :], st['gb'], t_ % 2)

            def prep_tr(tg_, tt_):
                transpose_tile(st, (tg_ * 4 + tt_) % 2, hTg, tt_ * 128, 'hTg', banks=(0, 1))

            pipelined = not moe
            for tg in range(4):
                for tt in range(4):
                    t = tg * 4 + tt
                    b = t % 2
                    if not pipelined:
                        prep_norm(tg, tt)
                        prep_tr(tg, tt)
                    elif tg == 0:
                        if tt == 0:
                            prep_norm(0, 0)
                        if tt + 1 < 4:
                            prep_norm(0, tt + 1)
                        prep_tr(0, tt)
                    if moe:
                        S.op('pool', lambda e, tt=tt, b=b: e.tensor_copy(out=acc[tt], in_=st['xt'][b]),
                             reads=[f'xt{b}'], writes=[f'acc{tt}'])

                        def mml(e, tt=tt):
                            for c in range(16):
                                ins = e.matmul(PS[2][:, 0:NE], lhsT=hTg[:, c, tt * 128:(tt + 1) * 128], rhs=wrs[:, c, :],
                                               start=(c == 0), stop=(c == 15))
                            return ins
                        S.op('pe', mml, reads=['hTg', 'wrs'], writes=['ps2'])
                        S.op('dve', lambda e, tt=tt: e.tensor_copy(out=lg[tt], in_=PS[2][:, 0:NE]), reads=['ps2'],
                             writes=[f'lg{tt}'])
                        S.op('dve', lambda e, tt=tt: e.max(out=mx[tt], in_=lg[tt]), reads=[f'lg{tt}'],
                             writes=[f'mx{tt}'])
                        S.op('dve', lambda e, tt=tt: e.tensor_tensor(out=gw[tt][:, 0:1], in0=mx[tt][:, 0:1],
                                                                     in1=mx[tt][:, 1:2], op=ALU.subtract),
                             reads=[f'mx{tt}'], writes=[f'gwa{tt}'])
                        S.op('act', lambda e, tt=tt: e.activation(out=gw[tt][:, 1:2], in_=gw[tt][:, 0:1],
                                                                  func=AF.Sigmoid),
                             reads=[f'gwa{tt}'], writes=[f'gwb{tt}'])
                        S.op('act', lambda e, tt=tt: e.activation(out=gw[tt][:, 2:3], in_=gw[tt][:, 0:1],
                                                                  func=AF.Sigmoid, scale=-1.0),
                             reads=[f'gwa{tt}'], writes=[f'gwc{tt}'])
                        S.op('dve', lambda e, tt=tt: e.tensor_scalar(out=gt[tt], in0=lg[tt], scalar1=mx[tt][:, 0:1],
                                                                     scalar2=gw[tt][:, 1:2], op0=ALU.is_equal,
                                                                     op1=ALU.mult),
                             reads=[f'lg{tt}', f'mx{tt}', f'gwb{tt}'], writes=[f'gt{tt}'])
                        S.op('dve', lambda e, tt=tt: e.tensor_scalar(out=gt2[tt], in0=lg[tt], scalar1=mx[tt][:, 1:2],
                                                                     scalar2=gw[tt][:, 2:3], op0=ALU.is_equal,
                                                                     op1=ALU.mult),
                             reads=[f'lg{tt}', f'mx{tt}', f'gwc{tt}'], writes=[f'gtb{tt}'])
                        S.op('dve', lambda e, tt=tt: e.tensor_tensor(out=gt[tt], in0=gt[tt], in1=gt2[tt], op=ALU.add),
                             reads=[f'gt{tt}', f'gtb{tt}'], writes=[f'gt{tt}'])
                for ex in range(NE if moe else 1):
                    if moe:
                        raise NotImplementedError("dense-evaluated MoE path retired (expert weights are host-permuted)")
                        Wd = we_d[ex].rearrange("(c p) n -> p c n", p=128)
                    else:
                        Wg = w_gu[:, 0:F].rearrange("(c p) n -> p c n", p=128)
                        Wu = w_gu[:, F:2 * F].rearrange("(c p) n -> p c n", p=128)
                        Wd = w_dn.rearrange("(c p) n -> p c n", p=128)
                    for jp in range(NJ // 2):
                        sg_, su_ = kk['gu'] % 4, (kk['gu'] + 1) % 4
                        kk['gu'] += 2
                        S.dma('pool', gus[sg_], Wg[:, :, jp * 256:(jp + 1) * 256], writes=[f'gus{sg_}'])
                        S.dma('pool', gus[su_], Wu[:, :, jp * 256:(jp + 1) * 256], writes=[f'gus{su_}'])
                        for jj in range(2):
                            j = jp * 2 + jj
                            pg = kk['ps'] % 2
                            pu = 2 + kk['ps'] % 2
                            kk['ps'] += 1

                            def mmg(e, sg_=sg_, jj=jj, pg=pg):
                                for c in range(16):
                                    ins = e.matmul(PS[pg], lhsT=gus[sg_][:, c, jj * 128:(jj + 1) * 128], rhs=hTg[:, c, :],
                                                   start=(c == 0), stop=(c == 15))
                                return ins

                            def mmu(e, su_=su_, jj=jj, pu=pu):
                                for c in range(16):
                                    ins = e.matmul(PS[pu], lhsT=gus[su_][:, c, jj * 128:(jj + 1) * 128], rhs=hTg[:, c, :],
                                                   start=(c == 0), stop=(c == 15))
                                return ins
                            S.op('pe', mmg, reads=[f'gus{sg_}', 'hTg'], writes=[f'ps{pg}'])
                            S.op('pe', mmu, reads=[f'gus{su_}', 'hTg'], writes=[f'ps{pu}'])
                            S.op('act', lambda e, pg=pg: e.activation(out=sgb[pg], in_=PS[pg], func=AF.Silu),
                                 reads=[f'ps{pg}'], writes=[f'sgb{pg}'])
                            S.op('dve', lambda e, pg=pg, pu=pu, j=j: e.tensor_tensor(out=heT[:, j, :], in0=sgb[pg],
                                                                                     in1=PS[pu], op=ALU.mult),
                                 reads=[f'sgb{pg}', f'ps{pu}'], writes=['heT'])
                    for n in range(4):
                        if pipelined and tg + 1 < 4:
                            prep_norm(tg + 1, n)
                        for p in range(NP):
                            wi = kk['wd'] % 3
                            kk['wd'] += 1
                            S.dma('pool', wdp[wi], Wd[:, p * CP:(p + 1) * CP, n * 512:(n + 1) * 512],
                                  writes=[f'wdp{wi}'])
                            for tt in range(4):
                                def mmd(e, wi=wi, tt=tt, p=p):
                                    for cc in range(CP):
                                        ins = e.matmul(PS[4 + tt], lhsT=heT[:, p * CP + cc, tt * 128:(tt + 1) * 128],
                                                       rhs=wdp[wi][:, cc, :], start=(p == 0 and cc == 0),
                                                       stop=(p == NP - 1 and cc == CP - 1))
                                    return ins
                                S.op('pe', mmd, reads=['heT', f'wdp{wi}'], writes=[f'ps{4 + tt}'])
                        for tt in range(4):
                            t = tg * 4 + tt
                            if moe:
                                S.op('dve', lambda e, tt=tt, n=n, ex=ex: e.scalar_tensor_tensor(
                                    out=acc[tt][:, n * 512:(n + 1) * 512], in0=PS[4 + tt], scalar=gt[tt][:, ex:ex + 1],
                                    in1=acc[tt][:, n * 512:(n + 1) * 512], op0=ALU.mult, op1=ALU.add),
                                    reads=[f'ps{4 + tt}', f'gt{tt}', f'acc{tt}'], writes=[f'acc{tt}'])
                            else:
                                r3 = kk['x'] % 3
                                kk['x'] += 1
                                S.dma('sp', xp[r3], src[t * 128:(t + 1) * 128, n * 512:(n + 1) * 512],
                                      writes=[f'fx{r3}'])
                                S.op('dve', lambda e, tt=tt, r3=r3: e.tensor_tensor(out=op_[r3], in0=PS[4 + tt],
                                                                                    in1=xp[r3], op=ALU.add),
                                     reads=[f'ps{4 + tt}', f'fx{r3}'], writes=[f'fo{r3}'])
                                S.dma('sp', dst[t * 128:(t + 1) * 128, n * 512:(n + 1) * 512], op_[r3],
                                      reads=[f'fo{r3}'], writes=['dst'])
                        if pipelined and tg + 1 < 4:
                            prep_tr(tg + 1, n)
                if moe:
                    for tt in range(4):
                        t = tg * 4 + tt
                        S.dma('sp', dst[t * 128:(t + 1) * 128, :], acc[tt], reads=[f'acc{tt}'], writes=['dst'])
            S.barrier()


    def moe_routed_phase(src, dst, gain):
        I32 = mybir.dt.int32
        WG, NCB = 896, 8
        NJ = FFE // 128
        with ExitStack() as es:
            S1i = es.enter_context(sbt("S1i", [128, 16], I32)).ap()
            S2i = es.enter_context(sbt("S2i", [128, 16], I32)).ap()
            W1 = es.enter_context(sbt("W1", [128, 16], F32)).ap()
            W2 = es.enter_context(sbt("W2", [128, 16], F32)).ap()
            IG = es.enter_context(sbt("IG", [128, 16], I32)).ap()
            ID = es.enter_context(sbt("ID", [128, 16], I32)).ap()
            with ExitStack() as es1:
                st = alloc_norm_state(es1)
                HTok = es1.enter_context(sbt("HTok", [128, 16, D], BF16)).ap()
                hT1 = [es1.enter_context(sbt(f"hT1{i}", [128, 16, 128], BF16)).ap() for i in range(2)]
                wrs = es1.enter_context(sbt("wrs", [128, 16, NE], BF16)).ap()
                trib = es1.enter_context(sbt("trib", [128, 128], BF16)).ap()
                pcol = es1.enter_context(sbt("pcol", [128, 1], F32)).ap()
                lg = es1.enter_context(sbt("lg", [128, 16, 8], F32)).ap()
                mx = es1.enter_context(sbt("mx", [128, 16, 8], F32)).ap()
                eq1 = es1.enter_context(sbt("eq1", [128, 16, 8], F32)).ap()
                eq2 = es1.enter_context(sbt("eq2", [128, 16, 8], F32)).ap()
                Mb = es1.enter_context(sbt("Mb", [128, 16, 8], BF16)).ap()
                CS = es1.enter_context(sbt("CS", [128, 16, 8], F32)).ap()
                TT = es1.enter_context(sbt("TT", [128, 16, 8], F32)).ap()
                OFF = es1.enter_context(sbt("OFF", [128, 16, 8], F32)).ap()
                Gp = es1.enter_context(sbt("Gp", [128, 16, 8], F32)).ap()
                tmp3 = es1.enter_context(sbt("tmp3", [128, 16, 8], F32)).ap()
                sm = es1.enter_context(sbt("sm", [128, 96], F32)).ap()
                S1f = es1.enter_context(sbt("S1f", [128, 16], F32)).ap()
                S2f = es1.enter_context(sbt("S2f", [128, 16], F32)).ap()
                IGf = es1.enter_context(sbt("IGf", [128, 16], F32)).ap()
                IDf = es1.enter_context(sbt("IDf", [128, 16], F32)).ap()
                S.dma('sp', st['gb'], gain.broadcast_to([128, D]), writes=['gb'])
                S.dma('pool', wrs, w_r.rearrange("(c p) n -> p c n", p=128), writes=['wrs'])
                S.dma('pool', trib, tri_d, writes=['trib'])
                S.dma('sp', pcol, pcol_d, writes=['pcol'])
                norm_tile(st, src[0:128, :], st['gb'], 0, dst=HTok[:, 0, :], dtag='HTok0')
                for t in range(NT):
                    b = t % 2
                    if t + 1 < NT:
                        norm_tile(st, src[(t + 1) * 128:(t + 2) * 128, :], st['gb'], (t + 1) % 2, dst=HTok[:, t + 1, :],
                                  dtag=f'HTok{t + 1}')
                    transpose_tile(st, b, hT1[b], 0, f'hT1{b}', banks=(6, 7), src=HTok[:, t, :], stag=f'HTok{t}')
                    pl = 4 + b

                    def mml(e, b=b, pl=pl):
                        for c in range(16):
                            ins = e.matmul(PS[pl][:, 0:NE], lhsT=hT1[b][:, c, :], rhs=wrs[:, c, :], start=(c == 0),
                                           stop=(c == 15))
                        return ins
                    S.op('pe', mml, reads=[f'hT1{b}', 'wrs'], writes=[f'ps{pl}'])
                    S.op('dve', lambda e, t=t, pl=pl: e.tensor_copy(out=lg[:, t, :], in_=PS[pl][:, 0:NE]),
                         reads=[f'ps{pl}'], writes=['lg'])
                    S.op('dve', lambda e, t=t: e.max(out=mx[:, t, :], in_=lg[:, t, :]), reads=['lg'], writes=['mx'])
                S.op('dve', lambda e: e.tensor_tensor(out=sm[:, 0:16], in0=mx[:, :, 0], in1=mx[:, :, 1],
                                                      op=ALU.subtract), reads=['mx'], writes=['sm_d'])
                S.op('act', lambda e: e.activation(out=W1, in_=sm[:, 0:16], func=AF.Sigmoid), reads=['sm_d'],
                     writes=['W1'])
                S.op('act', lambda e: e.activation(out=W2, in_=sm[:, 0:16], func=AF.Sigmoid, scale=-1.0),
                     reads=['sm_d'], writes=['W2'])
                S.op('dve', lambda e: e.tensor_tensor(out=eq1, in0=lg, in1=mx[:, :, 0:1].broadcast_to([128, 16, 8]),
                                                      op=ALU.is_equal), reads=['lg', 'mx'], writes=['eq1'])
                S.op('dve', lambda e: e.tensor_tensor(out=eq2, in0=lg, in1=mx[:, :, 1:2].broadcast_to([128, 16, 8]),
                                                      op=ALU.is_equal), reads=['lg', 'mx'], writes=['eq2'])
                S.op('dve', lambda e: e.tensor_tensor(out=Mb, in0=eq1, in1=eq2, op=ALU.add), reads=['eq1', 'eq2'],
                     writes=['Mb'])
                Mb2 = Mb.rearrange("p t e -> p (t e)")
                S.op('pe', lambda e: e.matmul(PS[0][:, 0:128], lhsT=trib, rhs=Mb2, start=True, stop=True),
                     reads=['Mb', 'trib'], writes=['ps0'])
                S.op('pe', lambda e: e.matmul(PS[1][:, 0:128], lhsT=onesb, rhs=Mb2, start=True, stop=True),
                     reads=['Mb', 'onesb'], writes=['ps1'])
                S.op('dve', lambda e: e.tensor_copy(out=CS.rearrange("p t e -> p (t e)"), in_=PS[0][:, 0:128]),
                     reads=['ps0'], writes=['CS'])
                S.op('dve', lambda e: e.tensor_copy(out=TT.rearrange("p t e -> p (t e)"), in_=PS[1][:, 0:128]),
                     reads=['ps1'], writes=['TT'])
                S.op('dve', lambda e: e.memset(OFF[:, 0, :], 0.0), writes=['OFF'])
                for t in range(1, NT):
                    S.op('dve', lambda e, t=t: e.tensor_tensor(out=OFF[:, t, :], in0=OFF[:, t - 1, :],
                                                               in1=TT[:, t - 1, :], op=ALU.add),
                         reads=['OFF', 'TT'], writes=['OFF'])
                cnt = sm[:, 16:24]
                nsl = sm[:, 24:32]
                cum = sm[:, 32:40]
                bas = sm[:, 40:48]
                t8 = sm[:, 48:56]
                ek = sm[:, 56:72]
                S.op('dve', lambda e: e.tensor_tensor(out=cnt, in0=OFF[:, 15, :], in1=TT[:, 15, :], op=ALU.add),
                     reads=['OFF', 'TT'], writes=['cnt'])
                S.op('dve', lambda e: e.tensor_scalar(out=nsl, in0=cnt, scalar1=0.0, scalar2=None, op0=ALU.is_gt),
                     reads=['cnt'], writes=['nsl'])
                for kk_ in (1, 2, 3):
                    S.op('dve', lambda e, kk_=kk_: e.tensor_scalar(out=t8, in0=cnt, scalar1=512.0 * kk_, scalar2=None,
                                                                   op0=ALU.is_gt), reads=['cnt'], writes=['t8'])
                    S.op('dve', lambda e: e.tensor_tensor(out=nsl, in0=nsl, in1=t8, op=ALU.add), reads=['nsl', 't8'],
                         writes=['nsl'])
                S.op('dve', lambda e: e.tensor_copy(out=cum[:, 0:1], in_=nsl[:, 0:1]), reads=['nsl'], writes=['cum'])
                for ee in range(1, NE):
                    S.op('dve', lambda e, ee=ee: e.tensor_tensor(out=cum[:, ee:ee + 1], in0=cum[:, ee - 1:ee],
                                                                 in1=nsl[:, ee:ee + 1], op=ALU.add),
                         reads=['cum', 'nsl'], writes=['cum'])
                S.op('dve', lambda e: e.tensor_tensor(out=bas, in0=cum, in1=nsl, op=ALU.subtract), reads=['cum', 'nsl'],
                     writes=['bas'])
                S.op('dve', lambda e: e.tensor_scalar(out=bas, in0=bas, scalar1=512.0, scalar2=-1.0, op0=ALU.mult,
                                                      op1=ALU.add), reads=['bas'], writes=['bas'])
                S.op('dve', lambda e: e.tensor_tensor(out=Gp, in0=CS, in1=OFF, op=ALU.add), reads=['CS', 'OFF'],
                     writes=['Gp'])
                S.op('dve', lambda e: e.tensor_tensor(out=Gp, in0=Gp, in1=bas.unsqueeze(1).broadcast_to([128, 16, 8]),
                                                      op=ALU.add), reads=['Gp', 'bas'], writes=['Gp'])
                S.op('dve', lambda e: e.tensor_tensor(out=tmp3, in0=Gp, in1=eq1, op=ALU.mult), reads=['Gp', 'eq1'],
                     writes=['tmp3'])
                S.op('dve', lambda e: e.tensor_reduce(out=S1f, in_=tmp3, axis=AX.X, op=ALU.add), reads=['tmp3'],
                     writes=['S1f'])
                S.op('dve', lambda e: e.tensor_tensor(out=tmp3, in0=Gp, in1=eq2, op=ALU.mult), reads=['Gp', 'eq2'],
                     writes=['tmp3'])
                S.op('dve', lambda e: e.tensor_reduce(out=S2f, in_=tmp3, axis=AX.X, op=ALU.add), reads=['tmp3'],
                     writes=['S2f'])
                S.op('dve', lambda e: e.tensor_copy(out=S1i, in_=S1f), reads=['S1f'], writes=['S1i'])
                S.op('dve', lambda e: e.tensor_copy(out=S2i, in_=S2f), reads=['S2f'], writes=['S2i'])
                for k_ in range(NSS):
                    S.op('dve', lambda e, k_=k_: e.tensor_scalar(out=t8, in0=cum, scalar1=float(k_), scalar2=None,
                                                                 op0=ALU.is_le), reads=['cum'], writes=['t8'])
                    S.op('dve', lambda e, k_=k_: e.tensor_reduce(out=ek[:, k_:k_ + 1], in_=t8, axis=AX.X, op=ALU.add),
                         reads=['t8'], writes=['ek'])
                S.op('dve', lambda e: e.tensor_scalar(out=ek, in0=ek, scalar1=float(NE - 1), scalar2=None, op0=ALU.min),
                     reads=['ek'], writes=['ek'])
                S.op('dve', lambda e: e.tensor_scalar(out=IGf, in0=ek, scalar1=float(NCB * 4 * 128), scalar2=None,
                                                      op0=ALU.mult), reads=['ek'], writes=['IGf'])
                S.op('dve', lambda e: e.scalar_tensor_tensor(out=IGf, in0=pcol.broadcast_to([128, 16]),
                                                             scalar=1.0, in1=IGf, op0=ALU.mult, op1=ALU.add),
                     reads=['IGf', 'pcol'], writes=['IGf'])
                S.op('dve', lambda e: e.tensor_scalar(out=IDf, in0=ek, scalar1=float(FFE * 2), scalar2=None,
                                                      op0=ALU.mult), reads=['ek'], writes=['IDf'])
                S.op('dve', lambda e: e.scalar_tensor_tensor(out=IDf, in0=pcol.broadcast_to([128, 16]), scalar=2.0,
                                                             in1=IDf, op0=ALU.mult, op1=ALU.add),
                     reads=['IDf', 'pcol'], writes=['IDf'])
                S.op('dve', lambda e: e.tensor_copy(out=IG, in_=IGf), reads=['IGf'], writes=['IG'])
                S.op('dve', lambda e: e.tensor_copy(out=ID, in_=IDf), reads=['IDf'], writes=['ID'])
                for t in range(NT):
                    S.idma(hs_d, HTok[:, t, :], S1i[:, t:t + 1], scatter=True, reads=[f'HTok{t}', 'S1i', 'hs_d'],
                           writes=[f'hs_s{t}a'])
                    S.idma(hs_d, HTok[:, t, :], S2i[:, t:t + 1], scatter=True, reads=[f'HTok{t}', 'S2i', 'hs_d'],
                           writes=[f'hs_s{t}b'])
                S.barrier()
            with ExitStack() as es2:
                hTs = es2.enter_context(sbt("hTs", [128, 16, 512], BF16)).ap()
                hst = [es2.enter_context(sbt(f"hst{i}", [128, D], BF16)).ap() for i in range(2)]
                heT = es2.enter_context(sbt("heT", [128, NJ, 512], BF16)).ap()
                slab = [es2.enter_context(sbt(f"slab{i}", [128, 16, WG], BF16)).ap() for i in range(3)]
                wdp = [es2.enter_context(sbt(f"wdq{i}", [128, 1024], BF16)).ap() for i in range(5)]
                sgs = es2.enter_context(sbt("sgs", [128, 7, 512], BF16)).ap()
                ost = [es2.enter_context(sbt(f"ost{i}", [128, 512], BF16)).ap() for i in range(8)]
                Wg2 = we_g
                Wu2 = we_u
                Wd2 = we_d.rearrange("e f (nh w) -> (e f nh) w", w=1024)
                loads = []
                for k_ in range(NSS):
                    for cb in range(NCB):
                        for which in (0, 1):
                            loads.append(('s', k_, cb, which))
                    for nh in range(2):
                        for j in range(NJ):
                            loads.append(('d', k_, nh, j))
                pos = {L: i for i, L in enumerate(loads)}
                stt = {'emitted': 0, 'ns': 0, 'nd': 0}
                slot_of = {}

                def emit_load(L):
                    if L[0] == 's':
                        _, k_, cb, which = L
                        r = stt['ns'] % 3
                        stt['ns'] += 1
                        slot_of[L] = r
                        Wsrc = Wg2 if which == 0 else Wu2
                        for cg in range(4):
                            S.idma(slab[r][:, cg * 4:(cg + 1) * 4, :].rearrange("p a b -> p (a b)"), Wsrc, IG[:, k_:k_ + 1],
                                   eoff=((cb * 4 + cg) * 128) * 4 * WG, reads=['IG'], writes=[f'slab{r}_{cg}'])
                    else:
                        _, k_, nh, j = L
                        r = stt['nd'] % 5
                        stt['nd'] += 1
                        slot_of[L] = r
                        S.idma(wdp[r], Wd2, ID[:, k_:k_ + 1], eoff=(j * 128 * 2 + nh) * 1024, reads=['ID'],
                               writes=[f'wdq{r}'])

                ring_cap = {'s': 3, 'd': 5}
                done = {'s': 0, 'd': 0}

                def ensure(L, ahead):
                    upto = min(len(loads), pos[L] + 1 + ahead)
                    while stt['emitted'] < upto:
                        nxt = loads[stt['emitted']]
                        ty = nxt[0]
                        if stt['n' + ty] - done[ty] >= ring_cap[ty]:
                            break
                        emit_load(nxt)
                        stt['emitted'] += 1

                ev = 0

                def load_hst(k_, tt):
                    b = tt % 2
                    S.dma('sp', hst[b], hs_d[(k_ * 4 + tt) * 128:(k_ * 4 + tt + 1) * 128, :], writes=[f'hst{b}'])

                load_hst(0, 0)
                load_hst(0, 1)
                for k_ in range(NSS):
                    for tt in range(4):
                        b = tt % 2
                        if tt >= 2:
                            load_hst(k_, tt)
                        transpose_tile(None, b, hTs, tt * 128, 'hTs', banks=(4, 5), src=hst[b], stag=f'hst{b}')
                    if k_ + 1 < NSS:
                        load_hst(k_ + 1, 0)
                        load_hst(k_ + 1, 1)
                    for cb in range(NCB):
                        for which in (0, 1):
                            L = ('s', k_, cb, which)
                            ensure(L, 2)
                            r = slot_of[L]
                            for jj in range(7):
                                j = cb * 7 + jj
                                pb = (0 if which == 0 else 2) + jj % 2

                                def mm(e, r=r, jj=jj, pb=pb):
                                    for c in range(16):
                                        ins = e.matmul(PS[pb], lhsT=slab[r][:, c, jj * 128:(jj + 1) * 128],
                                                       rhs=hTs[:, c, :], start=(c == 0), stop=(c == 15))
                                    return ins
                                S.op('pe', mm, reads=[f'slab{r}_{c}' for c in range(4)] + ['hTs'], writes=[f'ps{pb}'])
                                if which == 0:
                                    S.op('act', lambda e, pb=pb, jj=jj: e.activation(out=sgs[:, jj, :], in_=PS[pb],
                                                                                     func=AF.Silu),
                                         reads=[f'ps{pb}'], writes=[f'sgs{jj}'])
                                else:
                                    S.op('dve', lambda e, pb=pb, jj=jj, j=j: e.tensor_tensor(
                                        out=heT[:, j, :], in0=sgs[:, jj, :], in1=PS[pb], op=ALU.mult),
                                        reads=[f'sgs{jj}', f'ps{pb}'], writes=['heT'])
                            done['s'] += 1
                    for nh in range(2):
                        for j in range(NJ):
                            L = ('d', k_, nh, j)
                            ensure(L, 4)
                            r = slot_of[L]

                            def mmd(e, r=r, j=j):
                                for tt in range(4):
                                    for n2 in range(2):
                                        ins = e.matmul(PS[tt * 2 + n2], lhsT=heT[:, j, tt * 128:(tt + 1) * 128],
                                                       rhs=wdp[r][:, n2 * 512:(n2 + 1) * 512], start=(j == 0),
                                                       stop=(j == NJ - 1))
                                return ins
                            S.op('pe', mmd, reads=['heT', f'wdq{r}'], writes=[f'ps{i}' for i in range(8)])
                            done['d'] += 1
                        for tt in range(4):
                            for n2 in range(2):
                                o4 = ev % 8
                                ev += 1
                                pb = tt * 2 + n2
                                copy_op(evac_engine(), ost[o4], PS[pb], reads=[f'ps{pb}'], writes=[f'ost{o4}'])
                                r0 = k_ * 512 + tt * 128
                                c0 = nh * 1024 + n2 * 512
                                S.dma('sp', outs_d[r0:r0 + 128, c0:c0 + 512], ost[o4], reads=[f'ost{o4}'],
                                      writes=['outs_d'])
                S.barrier()
            with ExitStack() as es3:
                xt = [es3.enter_context(sbt(f"cx{i}", [128, D], F32)).ap() for i in range(2)]
                g1 = [es3.enter_context(sbt(f"cg1{i}", [128, D], BF16)).ap() for i in range(2)]
                g2 = [es3.enter_context(sbt(f"cg2{i}", [128, D], BF16)).ap() for i in range(2)]
                ac = [es3.enter_context(sbt(f"cac{i}", [128, D], F32)).ap() for i in range(2)]
                for t in range(NT):
                    b = t % 2
                    S.dma('sp', xt[b], src[t * 128:(t + 1) * 128, :], writes=[f'cx{b}'])
                    S.idma(g1[b], outs_d, S1i[:, t:t + 1], reads=['S1i'], writes=[f'cg1{b}'])
                    S.idma(g2[b], outs_d, S2i[:, t:t + 1], reads=['S2i'], writes=[f'cg2{b}'])
                    S.op('dve', lambda e, b=b, t=t: e.scalar_tensor_tensor(out=ac[b], in0=g1[b], scalar=W1[:, t:t + 1],
                                                                           in1=xt[b], op0=ALU.mult, op1=ALU.add),
                         reads=[f'cg1{b}', f'cx{b}', 'W1'], writes=[f'cac{b}'])
                    S.op('dve', lambda e, b=b, t=t: e.scalar_tensor_tensor(out=ac[b], in0=g2[b], scalar=W2[:, t:t + 1],
                                                                           in1=ac[b], op0=ALU.mult, op1=ALU.add),
                         reads=[f'cg2{b}', f'cac{b}', 'W2'], writes=[f'cac{b}'])
                    S.dma('sp', dst[t * 128:(t + 1) * 128, :], ac[b], reads=[f'cac{b}'], writes=['dst'])
                S.barrier()

    def gmlp_phase(src, dst):
        with ExitStack() as es:
            HT = es.enter_context(sbt("HT", [128, 16, T], BF16)).ap()
            with ExitStack() as es2:
                st = alloc_norm_state(es2)
                norm_transpose_all(st, src, g_l1_mix, HT)
                S.barrier()
            es3 = ExitStack()
            vsb = es3.enter_context(sbt("vsb", [128, 16, D], BF16)).ap()
            wb = [es3.enter_context(sbt(f"gw{i}", [128, 16, 512], BF16)).ap() for i in range(2)]
            vgb = es3.enter_context(sbt("vgb", [128, D], F32)).ap()
            wsT = es3.enter_context(sbt("wsT", [128, 16, 128], BF16)).ap()
            bsb = es3.enter_context(sbt("bsb", [128, 16, 128], F32)).ap()
            tmp = [es3.enter_context(sbt(f"gtmp{i}", [128, 512], F32)).ap() for i in range(2)]
            junk = es3.enter_context(sbt("gjunk", [128, 512], BF16)).ap()
            ssv = es3.enter_context(sbt("ssv", [128, 64], F32)).ap()
            ss1 = es3.enter_context(sbt("ss1", [128, 48], F32)).ap()
            es4 = ExitStack()
            wsn = es4.enter_context(sbt("wsn", [128, 16, 128], BF16)).ap()

            S.dma('sp', vgb, g_v.broadcast_to([128, D]), writes=['vgb'])
            S.dma('sp', bsb.rearrange("p g t -> p (g t)"), b_s.broadcast_to([128, 16 * 128]), writes=['bsb'])
            S.dma('pool', wsn, w_s.rearrange("g t s -> t g s"), writes=['wsn'])
            for hf in range(2):
                def trw(e, hf=hf):
                    for c in range(8):
                        ins = e.transpose(out=PSB[hf][:, c * 128:(c + 1) * 128], in_=wsn[:, hf * 8 + c, :], identity=idb)
                    return ins
                S.op('pe', trw, reads=['wsn', 'idb'], writes=[f'ps{hf}'])
                copy_op('dve', wsT[:, hf * 8:(hf + 1) * 8, :], PSB[hf].rearrange("p (c t) -> p c t", c=8),
                        reads=[f'ps{hf}'], writes=['wsT'])
            S.barrier()
            es4.close()
            ugb = [es3.enter_context(sbt(f"ugb{i}", [128, 512], F32)).ap() for i in range(2)]
            vpb = [es3.enter_context(sbt(f"vpb{i}", [128, 512], F32)).ap() for i in range(2)]
            yTj = [es3.enter_context(sbt(f"yTj{i}", [128, T], BF16)).ap() for i in range(2)]
            Wv_ = w_in.rearrange("(c p) n -> p c n", p=128)
            k = 0
            for n in range(4):
                wbn = n % 2
                S.dma('pool', wb[wbn], Wv_[:, :, D + n * 512:D + (n + 1) * 512], writes=[f'gw{wbn}'])
                for t in range(NT):
                    pb = 2 + k % 4
                    r3 = k % 2
                    k += 1

                    def mm(e, t=t, wbn=wbn, pb=pb):
                        for c in range(16):
                            ins = e.matmul(PS[pb], lhsT=HT[:, c, t * 128:(t + 1) * 128], rhs=wb[wbn][:, c, :],
                                           start=(c == 0), stop=(c == 15))
                        return ins
                    S.op('pe', mm, reads=[f'HT{t // 4}', f'gw{wbn}'], writes=[f'ps{pb}'])
                    S.op('act', lambda e, pb=pb, r3=r3: e.activation(out=tmp[r3], in_=PS[pb], func=AF.Gelu_apprx_tanh),
                         reads=[f'ps{pb}'], writes=[f'gtmp{r3}'])
                    S.op('act', lambda e, r3=r3, t=t, n=n: e.activation(out=junk, in_=tmp[r3], func=AF.Square,
                                                                        accum_out=ssv[:, t * 4 + n:t * 4 + n + 1]),
                         reads=[f'gtmp{r3}'], writes=['gjunk', 'ssv'])
                    S.op('dve', lambda e, r3=r3, t=t, n=n: e.tensor_copy(out=vsb[:, t, n * 512:(n + 1) * 512],
                                                                         in_=tmp[r3]),
                         reads=[f'gtmp{r3}'], writes=[f'vsb{t}'])
            S.op('dve', lambda e: e.tensor_reduce(out=ss1[:, 0:16], in_=ssv.rearrange("p (t n) -> p t n", n=4),
                                                  axis=AX.X, op=ALU.add), reads=['ssv'], writes=['ss1a'])
            S.op('act', lambda e: e.activation(out=ss1[:, 16:32], in_=ss1[:, 0:16], func=AF.Sqrt, scale=1.0 / D,
                                               bias=EPS), reads=['ss1a'], writes=['ss1b'])
            S.op('dve', lambda e: e.reciprocal(out=ss1[:, 32:48], in_=ss1[:, 16:32]), reads=['ss1b'], writes=['ss1c'])
            for t in range(NT):
                S.op('dve', lambda e, t=t: e.scalar_tensor_tensor(out=vsb[:, t, :], in0=vsb[:, t, :],
                                                                  scalar=ss1[:, 32 + t:33 + t], in1=vgb, op0=ALU.mult,
                                                                  op1=ALU.mult),
                     reads=[f'vsb{t}', 'ss1c', 'vgb'], writes=[f'vsb{t}'])
            for jp in range(8):
                wbn = jp % 2
                S.dma('pool', wb[wbn][:, :, 0:256], Wv_[:, :, jp * 256:(jp + 1) * 256], writes=[f'gw{wbn}'])
                for jj in range(2):
                    j = jp * 2 + jj
                    yb = j % 2
                    for tg in range(4):
                        pu = k % 2
                        pv = 2 + k % 2
                        k += 1

                        def mmu(e, wbn=wbn, jj=jj, tg=tg, pu=pu):
                            for c in range(16):
                                ins = e.matmul(PS[pu], lhsT=wb[wbn][:, c, jj * 128:(jj + 1) * 128],
                                               rhs=HT[:, c, tg * 512:(tg + 1) * 512], start=(c == 0), stop=(c == 15))
                            return ins
                        S.op('pe', mmu, reads=[f'gw{wbn}', f'HT{tg}'], writes=[f'ps{pu}'])

                        def mmv(e, j=j, tg=tg, pv=pv):
                            for tt in range(4):
                                ins = e.matmul(PS[pv][:, tt * 128:(tt + 1) * 128],
                                               lhsT=vsb[:, tg * 4 + tt, j * 128:(j + 1) * 128], rhs=wsT[:, j, :],
                                               start=True, stop=True)
                            return ins
                        S.op('pe', mmv, reads=[f'vsb{tg * 4 + tt}' for tt in range(4)] + ['wsT'], writes=[f'ps{pv}'])
                        S.op('act', lambda e, pu=pu: e.activation(out=ugb[pu], in_=PS[pu], func=AF.Gelu_apprx_tanh),
                             reads=[f'ps{pu}'], writes=[f'ugb{pu}'])
                        S.op('dve', lambda e, pv=pv, pu=pu, j=j: e.tensor_tensor(
                            out=vpb[pu].rearrange("p (a b) -> p a b", a=4),
                            in0=PS[pv].rearrange("p (a b) -> p a b", a=4),
                            in1=bsb[:, j:j + 1, :].broadcast_to([128, 4, 128]), op=ALU.add),
                            reads=[f'ps{pv}', 'bsb'], writes=[f'vpb{pu}'])
                        S.op('dve', lambda e, pu=pu, yb=yb, tg=tg: e.tensor_tensor(
                            out=yTj[yb][:, tg * 512:(tg + 1) * 512], in0=vpb[pu], in1=ugb[pu], op=ALU.mult),
                            reads=[f'vpb{pu}', f'ugb{pu}'], writes=[f'yTj{yb}'])
                    S.dma('sp', msc[j], yTj[yb], reads=[f'yTj{yb}'], writes=['msc'])
            S.barrier()
            es3.close()
            proj_residual(HT, w_out, src, dst)

    S.barrier()
    if stage >= 1:
        attention_phase(x_in, hs_d if stage >= 4 else None)
    last = xa
    if stage >= 2:
        ffn_phase(xa, xb, g_l0_ffn, moe=False)
        last = xb
    if stage >= 3:
        gmlp_phase(xb, xc)
        last = xc
    if stage >= 4:
        moe_routed_phase(xc, y_out, g_l1_ffn)
    else:
        with ExitStack() as es:
            cp = [es.enter_context(sbt(f"cp{i}", [128, D], F32)).ap() for i in range(2)]
            for t in range(NT):
                S.dma('sp', cp[t % 2], last[t * 128:(t + 1) * 128, :], reads=['dst'], writes=[f'cp{t % 2}'])
                S.dma('sp', y_out[t * 128:(t + 1) * 128, :], cp[t % 2], reads=[f'cp{t % 2}'], writes=['y'])
    S.barrier()
    return nc


def make_consts():
    ident = np.eye(128, dtype=np.float32)
    slopes = 2.0 ** (-8.0 * np.arange(1, 17, dtype=np.float64) / 16)
    s = np.arange(128)[:, None]
    q = np.arange(128)[None, :]
    bias = np.zeros((128, 3, 16, 128), np.float32)
    for j in range(3):
        delta = q - s + (1 - j) * 128
        dist = np.abs(delta)
        valid = dist <= 128
        for h in range(16):
            b = np.where(valid, -slopes[h] * dist, -1e30) / SCALE
            bias[:, j, h, :] = b.astype(np.float32)
    return ident, bias.reshape(128, 3 * 16 * 128)


def permute_gu(w):
    e = w.shape[0]
    v = w.reshape(e, 4, 4, 128, 8, 896)
    v = np.ascontiguousarray(v.transpose(0, 4, 1, 3, 2, 5))
    return v.reshape(e * 8 * 4 * 128, 4 * 896)


_NC_CACHE = {}


def kernel(**inputs):
    stage = int(inputs.pop('_stage', 99))
    ncores = int(inputs.pop('_ncores', NCORES))
    if stage not in _NC_CACHE:
        _NC_CACHE[stage] = build_program(stage)
    nc = _NC_CACHE[stage]
    ident, bias = make_consts()
    f = lambda a: np.ascontiguousarray(np.asarray(a, dtype=np.float32))
    shared = {
        'l0_mix_norm': f(inputs['l0_mix_norm']).reshape(1, D),
        'l0_w_qkv': f(inputs['l0_w_qkv']),
        'l0_q_norm': f(inputs['l0_q_norm']).reshape(128, 1),
        'l0_k_norm': f(inputs['l0_k_norm']).reshape(128, 1),
        'l0_sink': f(inputs['l0_sink']).reshape(1, 16),
        'l0_w_o': f(inputs['l0_w_o']),
        'l0_ffn_norm': f(inputs['l0_ffn_norm']).reshape(1, D),
        'l0_w_gate_up': f(inputs['l0_w_gate_up']),
        'l0_w_down': f(inputs['l0_w_down']),
        'l1_mix_norm': f(inputs['l1_mix_norm']).reshape(1, D),
        'l1_w_in': f(inputs['l1_w_in']),
        'l1_v_norm': f(inputs['l1_v_norm']).reshape(1, D),
        'l1_w_s': f(inputs['l1_w_s']),
        'l1_b_s': f(inputs['l1_b_s']).reshape(1, 16 * 128),
        'l1_w_out': f(inputs['l1_w_out']),
        'l1_ffn_norm': f(inputs['l1_ffn_norm']).reshape(1, D),
        'l1_w_router': f(inputs['l1_w_router']) if stage >= 4 else None,
        'l1_we_gate': permute_gu(f(inputs['l1_we_gate'])) if stage >= 4 else None,
        'l1_we_up': permute_gu(f(inputs['l1_we_up'])) if stage >= 4 else None,
        'l1_we_down': f(inputs['l1_we_down']) if stage >= 4 else None,
        'c_ident': ident,
        'c_bias': bias,
        'c_tri': np.triu(np.ones((128, 128), np.float32)),
        'c_pcol': np.arange(128, dtype=np.float32).reshape(128, 1),
    }
    if stage < 4:
        for kname in ('l1_w_router', 'l1_we_gate', 'l1_we_up', 'l1_we_down', 'c_tri', 'c_pcol'):
            shared.pop(kname)
    x = f(inputs['x'])
    in_maps = []
    for c in range(ncores):
        m = dict(shared)
        m['x'] = x[c]
        in_maps.append(m)
    res = run_bass_kernel_spmd(nc, in_maps, core_ids=list(range(ncores)))
    return np.stack([np.asarray(r['y'], dtype=np.float32) for r in res.results], axis=0)
```

```python
import numpy as np
from contextlib import ExitStack
import concourse.bass as bass
import concourse.mybir as mybir
from concourse.bass_utils import run_bass_kernel_spmd

F32 = mybir.dt.float32
BF16 = mybir.dt.bfloat16
AF = mybir.ActivationFunctionType
ALU = mybir.AluOpType
AX = mybir.AxisListType

D = 2048
T = 2048
NT = 16
NCORES = 8
FF0 = 5632
FFE = 7168
NE = 8
EPS = 1e-6
SCALE = 128.0 ** -0.5


class Sched:
    def __init__(self, nc, ndma=20):
        self.nc = nc
        self.eng = {'pe': nc.tensor, 'act': nc.scalar, 'dve': nc.vector, 'pool': nc.gpsimd, 'sp': nc.sync}
        self.sem = {e: nc.alloc_semaphore(f"s_{e}") for e in ('pe', 'act', 'dve', 'pool')}
        self.cnt = {e: 0 for e in self.sem}
        self.seen = {e: {} for e in self.eng}
        self.last_w = {}
        self.readers = {}
        self.dsem = {q: [nc.alloc_semaphore(f"d_{q}_{i}") for i in range(ndma)] for q in ('sp', 'pool')}
        self.dcnt = {q: 0 for q in self.dsem}
        self.dval = {q: [0] * ndma for q in self.dsem}
        self.semkey = {}

    def _wait(self, e, tok):
        sem, val = tok
        if e == 'pe' and sem is self.sem['pe']:
            return
        k = id(sem)
        self.semkey[k] = sem
        if self.seen[e].get(k, 0) < val:
            self.eng[e].wait_ge(sem, val)
            self.seen[e][k] = val

    def _deps(self, e, reads, writes):
        for b in reads:
            w = self.last_w.get(b)
            if w:
                self._wait(e, w)
        for b in writes:
            w = self.last_w.get(b)
            if w:
                self._wait(e, w)
            for r in self.readers.get(b, ()):
                self._wait(e, r)

    def _commit(self, tok, reads, writes):
        for b in reads:
            self.readers.setdefault(b, []).append(tok)
        for b in writes:
            self.last_w[b] = tok
            self.readers[b] = []

    def op(self, e, fn, reads=(), writes=()):
        self._deps(e, reads, writes)
        ins = fn(self.eng[e])
        self.cnt[e] += 1
        ins.then_inc(self.sem[e], 1)
        tok = (self.sem[e], self.cnt[e])
        self._commit(tok, reads, writes)
        return tok

    def dma(self, q, out, in_, reads=(), writes=()):
        self._deps(q, reads, writes)
        ring = self.dsem[q]
        i = self.dcnt[q] % len(ring)
        self.dcnt[q] += 1
        sem = ring[i]
        if self.dval[q][i] > 0:
            self._wait(q, (sem, self.dval[q][i]))
        self.eng[q].dma_start(out=out, in_=in_).then_inc(sem, 16)
        self.dval[q][i] += 16
        tok = (sem, self.dval[q][i])
        self._commit(tok, reads, writes)
        return tok

    def idma(self, out, in_, idxcol, eoff=0, scatter=False, reads=(), writes=()):
        q = 'pool'
        self._deps(q, reads, writes)
        ring = self.dsem[q]
        i = self.dcnt[q] % len(ring)
        self.dcnt[q] += 1
        sem = ring[i]
        if self.dval[q][i] > 0:
            self._wait(q, (sem, self.dval[q][i]))
        off = bass.IndirectOffsetOnAxis(ap=idxcol, axis=0)
        if scatter:
            ins = self.nc.gpsimd.indirect_dma_start(out=out, out_offset=off, in_=in_, in_offset=None,
                                                    element_offset=eoff)
        else:
            ins = self.nc.gpsimd.indirect_dma_start(out=out, out_offset=None, in_=in_, in_offset=off,
                                                    element_offset=eoff)
        ins.then_inc(sem, 16)
        self.dval[q][i] += 16
        tok = (sem, self.dval[q][i])
        self._commit(tok, reads, writes)
        return tok

    def barrier(self):
        toks = [(self.sem[e], self.cnt[e]) for e in self.sem if self.cnt[e] > 0]
        for q in self.dsem:
            for i, s in enumerate(self.dsem[q]):
                if self.dval[q][i] > 0:
                    toks.append((s, self.dval[q][i]))
        for e in self.eng:
            for tok in toks:
                self._wait(e, tok)
        self.last_w = {}
        self.readers = {}


def build_program(stage=99):
    nc = bass.Bass("TRN2", target_bir_lowering=False)

    def din(name, shape):
        return nc.dram_tensor(name, list(shape), F32, kind="ExternalInput").ap()

    x_in = din("x", [T, D])
    g_l0_mix = din("l0_mix_norm", [1, D])
    w_qkv = din("l0_w_qkv", [D, 3072])
    g_q = din("l0_q_norm", [128, 1])
    g_k = din("l0_k_norm", [128, 1])
    sink = din("l0_sink", [1, 16])
    w_o = din("l0_w_o", [D, D])
    g_l0_ffn = din("l0_ffn_norm", [1, D])
    w_gu = din("l0_w_gate_up", [D, 2 * FF0])
    w_dn = din("l0_w_down", [FF0, D])
    g_l1_mix = din("l1_mix_norm", [1, D])
    w_in = din("l1_w_in", [D, 2 * D])
    g_v = din("l1_v_norm", [1, D])
    w_s = din("l1_w_s", [16, 128, 128])
    b_s = din("l1_b_s", [1, 16 * 128])
    w_out = din("l1_w_out", [D, D])
    g_l1_ffn = din("l1_ffn_norm", [1, D])
    if stage >= 4:
        w_r = din("l1_w_router", [D, NE])
        we_g = din("l1_we_gate", [NE, D, FFE])
        we_u = din("l1_we_up", [NE, D, FFE])
        we_d = din("l1_we_down", [NE, FFE, D])
    ident_d = din("c_ident", [128, 128])
    if stage >= 4:
        tri_d = din("c_tri", [128, 128])
        pcol_d = din("c_pcol", [128, 1])
    bias_d = din("c_bias", [128, 3 * 16 * 128])
    y_out = nc.dram_tensor("y", [T, D], F32, kind="ExternalOutput").ap()

    xa = nc.dram_tensor("xa", [T, D], F32).ap()
    xb = nc.dram_tensor("xb", [T, D], F32).ap()
    xc = nc.dram_tensor("xc", [T, D], F32).ap()
    msc = nc.dram_tensor("msc", [16, 128, T], BF16).ap()
    NSS = 15
    hs_d = nc.dram_tensor("hs_d", [NSS * 512, D], BF16).ap()
    outs_d = nc.dram_tensor("outs_d", [NSS * 512, D], BF16).ap()

    S = Sched(nc)
    PS = [nc.alloc_psum_tensor(f"ps{i}", [128, 512], F32).ap() for i in range(8)]
    PSB = [p.bitcast(BF16) for p in PS]

    idb = nc.alloc_sbuf_tensor("idb", [128, 128], BF16).ap()
    onesb = nc.alloc_sbuf_tensor("onesb", [128, 128], BF16).ap()
    ones32 = nc.alloc_sbuf_tensor("ones32", [128, 128], F32).ap()
    S.dma('pool', idb, ident_d, writes=['idb'])
    S.op('dve', lambda e: e.memset(onesb, 1.0), writes=['onesb'])
    S.op('dve', lambda e: e.memset(ones32, 1.0), writes=['ones32'])

    rr = {'n': 0, 'u': 0}

    def sbt(name, shape, dt):
        rr['u'] += 1
        return nc.sbuf_tensor(f"{name}_{rr['u']}", shape, dt)

    def evac_engine():
        rr['n'] += 1
        return 'act' if rr['n'] % 2 == 0 else 'dve'

    def copy_op(e, out, in_, reads, writes):
        if e == 'act':
            S.op('act', lambda en: en.activation(out=out, in_=in_, func=AF.Copy), reads=reads, writes=writes)
        else:
            S.op(e, lambda en: en.tensor_copy(out=out, in_=in_), reads=reads, writes=writes)

    def norm_tile(st, src_rows, gb, b, dst=None, dtag=None):
        xt, ht, junk, ss = st['xt'][b], (st['ht'][b] if dst is None else dst), st['junk'], st['ss'][b]
        S.dma('sp', xt, src_rows, writes=[f'xt{b}'])
        S.op('act', lambda e: e.activation(out=junk, in_=xt, func=AF.Square, accum_out=ss[:, 0:1]),
             reads=[f'xt{b}'], writes=['junk', f'ss{b}'])
        S.op('act', lambda e: e.activation(out=ss[:, 1:2], in_=ss[:, 0:1], func=AF.Sqrt, scale=1.0 / D, bias=EPS),
             reads=[f'ss{b}'], writes=[f'ssb{b}'])
        S.op('dve', lambda e: e.reciprocal(out=ss[:, 2:3], in_=ss[:, 1:2]), reads=[f'ssb{b}'], writes=[f'ssc{b}'])
        S.op('dve', lambda e: e.scalar_tensor_tensor(out=ht, in0=xt, scalar=ss[:, 2:3], in1=gb, op0=ALU.mult,
                                                     op1=ALU.mult),
             reads=[f'xt{b}', f'ssc{b}', 'gb'], writes=[dtag or f'ht{b}'])

    def transpose_tile(st, b, dstT, col0, dtag, banks=(6, 7), src=None, stag=None):
        ht = st['ht'][b] if src is None else src
        for hf in range(2):
            pb = banks[hf]

            def tr(e, hf=hf, pb=pb):
                for c in range(8):
                    ins = e.transpose(out=PSB[pb][:, c * 128:(c + 1) * 128],
                                      in_=ht[:, (hf * 8 + c) * 128:(hf * 8 + c + 1) * 128], identity=idb)
                return ins
            S.op('pe', tr, reads=[stag or f'ht{b}', 'idb'], writes=[f'ps{pb}'])
            copy_op(evac_engine(), dstT[:, hf * 8:(hf + 1) * 8, col0:col0 + 128],
                    PSB[pb].rearrange("p (c t) -> p c t", c=8), reads=[f'ps{pb}'], writes=[dtag])

    def alloc_norm_state(es):
        st = {}
        st['xt'] = [es.enter_context(sbt(f"xt{i}", [128, D], F32)).ap() for i in range(2)]
        st['ht'] = [es.enter_context(sbt(f"ht{i}", [128, D], BF16)).ap() for i in range(2)]
        st['junk'] = es.enter_context(sbt("junk", [128, D], BF16)).ap()
        st['ss'] = [es.enter_context(sbt(f"ss{i}", [128, 4], F32)).ap() for i in range(2)]
        st['gb'] = es.enter_context(sbt("gb", [128, D], F32)).ap()
        return st

    def norm_transpose_all(st, src, gain, HT):
        S.dma('sp', st['gb'], gain.broadcast_to([128, D]), writes=['gb'])
        norm_tile(st, src[0:128, :], st['gb'], 0)
        for t in range(NT):
            if t + 1 < NT:
                norm_tile(st, src[(t + 1) * 128:(t + 2) * 128, :], st['gb'], (t + 1) % 2)
            transpose_tile(st, t % 2, HT, t * 128, f'HT{t // 4}')

    def proj_residual(HT, W, src, dst):
        with ExitStack() as es:
            wb = [es.enter_context(sbt(f"prw{i}", [128, 16, 512], BF16)).ap() for i in range(2)]
            xp = [es.enter_context(sbt(f"prx{i}", [128, 512], F32)).ap() for i in range(3)]
            op_ = [es.enter_context(sbt(f"pro{i}", [128, 512], F32)).ap() for i in range(3)]
            mscv = msc.rearrange("c p t -> p c t")
            for g in range(4):
                S.dma('sp', HT[:, :, g * 512:(g + 1) * 512], mscv[:, :, g * 512:(g + 1) * 512], reads=['msc'],
                      writes=[f'HTm{g}'])
            Wv = W.rearrange("(c p) n -> p c n", p=128)
            k = 0
            for n in range(4):
                wbn = n % 2
                S.dma('pool', wb[wbn], Wv[:, :, n * 512:(n + 1) * 512], writes=[f'prw{wbn}'])
                for t in range(NT):
                    pb = k % 4
                    r3 = k % 3
                    k += 1
                    S.dma('sp', xp[r3], src[t * 128:(t + 1) * 128, n * 512:(n + 1) * 512], writes=[f'prx{r3}'])

                    def mm(e, t=t, wbn=wbn, pb=pb):
                        for c in range(16):
                            ins = e.matmul(PS[pb], lhsT=HT[:, c, t * 128:(t + 1) * 128], rhs=wb[wbn][:, c, :],
                                           start=(c == 0), stop=(c == 15))
                        return ins
                    S.op('pe', mm, reads=[f'HTm{t // 4}', f'prw{wbn}'], writes=[f'ps{pb}'])
                    S.op('dve', lambda e, pb=pb, r3=r3: e.tensor_tensor(out=op_[r3], in0=PS[pb], in1=xp[r3],
                                                                        op=ALU.add),
                         reads=[f'ps{pb}', f'prx{r3}'], writes=[f'pro{r3}'])
                    S.dma('sp', dst[t * 128:(t + 1) * 128, n * 512:(n + 1) * 512], op_[r3], reads=[f'pro{r3}'],
                          writes=['dst'])
            S.barrier()

    def attention_phase(src, zero_fill_target=None):
        with ExitStack() as es:
            HT = es.enter_context(sbt("HT", [128, 16, T], BF16)).ap()
            with ExitStack() as es2:
                st = alloc_norm_state(es2)
                norm_transpose_all(st, src, g_l0_mix, HT)
                S.barrier()
            es3 = ExitStack()
            wq = es3.enter_context(sbt("wq", [128, 16, 512], BF16)).ap()
            wk = es3.enter_context(sbt("wk", [128, 16, 128], BF16)).ap()
            wv = es3.enter_context(sbt("wv", [128, 16, 128], BF16)).ap()
            qT = es3.enter_context(sbt("qT", [128, 16, 4, 128], BF16)).ap()
            kT = es3.enter_context(sbt("kT", [128, T], BF16)).ap()
            Vg = es3.enter_context(sbt("Vg", [128, 16, 128], BF16)).ap()
            aT = es3.enter_context(sbt("aT", [128, 4, T], BF16)).ap()
            biasT = es3.enter_context(sbt("biasT", [128, 3, 16, 128], BF16)).ap()
            sexp = es3.enter_context(sbt("sexp", [128, 16], F32)).ap()
            sinkexp = es3.enter_context(sbt("sinkexp", [128, 16, 128], F32)).ap()
            gq = es3.enter_context(sbt("gq", [128, 1], F32)).ap()
            gk = es3.enter_context(sbt("gk", [128, 1], F32)).ap()
            sqb = [es3.enter_context(sbt(f"sqb{i}", [128, 512], F32)).ap() for i in range(2)]
            stdb = [es3.enter_context(sbt(f"stdb{i}", [128, 512], F32)).ap() for i in range(2)]
            rsb = [es3.enter_context(sbt(f"rsb{i}", [128, 512], F32)).ap() for i in range(2)]
            PT = [es3.enter_context(sbt(f"PT{i}", [128, 512], BF16)).ap() for i in range(6)]
            denb = [es3.enter_context(sbt(f"denb{i}", [128, 512], F32)).ap() for i in range(2)]
            rdb = [es3.enter_context(sbt(f"rdb{i}", [128, 512], F32)).ap() for i in range(2)]

            S.dma('pool', biasT, bias_d.rearrange("p (j h q) -> p j h q", j=3, h=16), writes=['biasT'])
            S.dma('sp', gq, g_q, writes=['gq'])
            S.dma('sp', gk, g_k, writes=['gk'])
            S.dma('sp', sexp, sink.broadcast_to([128, 16]), writes=['sexp'])
            if zero_fill_target is not None:
                ztz = es3.enter_context(sbt("ztz", [128, D], BF16)).ap()
                S.op('pool', lambda e: e.memset(ztz, 0.0), writes=['ztz'])
                for i in range(zero_fill_target.shape[0] // 128):
                    S.dma('sp', zero_fill_target[i * 128:(i + 1) * 128, :], ztz, reads=['ztz'], writes=['hs_zero'])
            S.op('act', lambda e: e.activation(out=sexp, in_=sexp, func=AF.Exp), reads=['sexp'], writes=['sexp'])
            S.op('dve', lambda e: e.tensor_copy(out=sinkexp, in_=sexp.unsqueeze(2).broadcast_to([128, 16, 128])),
                 reads=['sexp'], writes=['sinkexp'])
            Wv_ = w_qkv.rearrange("(c p) n -> p c n", p=128)
            kq = 0
            for kv in range(4):
                S.dma('pool', wq, Wv_[:, :, kv * 512:(kv + 1) * 512], writes=['wq'])
                S.dma('pool', wk, Wv_[:, :, 2048 + kv * 128:2048 + (kv + 1) * 128], writes=['wk'])
                S.dma('pool', wv, Wv_[:, :, 2560 + kv * 128:2560 + (kv + 1) * 128], writes=['wv'])
                for tg in range(4):
                    for hd in range(5):
                        pb = kq % 2
                        pb2 = 2 + kq % 2
                        r2 = kq % 2
                        kq += 1
                        wsrc = wq[:, :, hd * 128:(hd + 1) * 128] if hd < 4 else wk
                        wtag = 'wq' if hd < 4 else 'wk'

                        def mm(e, wsrc=wsrc, pb=pb, tg=tg):
                            for c in range(16):
                                ins = e.matmul(PS[pb], lhsT=wsrc[:, c, :], rhs=HT[:, c, tg * 512:(tg + 1) * 512],
                                               start=(c == 0), stop=(c == 15))
                            return ins
                        S.op('pe', mm, reads=[wtag, f'HT{tg}'], writes=[f'ps{pb}'])
                        S.op('act', lambda e, pb=pb, r2=r2: e.activation(out=sqb[r2], in_=PS[pb], func=AF.Square),
                             reads=[f'ps{pb}'], writes=[f'sqb{r2}'])
                        S.op('pe', lambda e, pb2=pb2, r2=r2: e.matmul(PS[pb2], lhsT=ones32, rhs=sqb[r2], start=True,
                                                                      stop=True),
                             reads=[f'sqb{r2}', 'ones32'], writes=[f'ps{pb2}'])
                        S.op('act', lambda e, pb2=pb2, r2=r2: e.activation(out=stdb[r2], in_=PS[pb2], func=AF.Sqrt,
                                                                           scale=1.0 / 128, bias=EPS),
                             reads=[f'ps{pb2}'], writes=[f'stdb{r2}'])
                        S.op('dve', lambda e, r2=r2: e.reciprocal(out=rsb[r2], in_=stdb[r2]), reads=[f'stdb{r2}'],
                             writes=[f'rsb{r2}'])
                        if hd < 4:
                            S.op('dve', lambda e, pb=pb, r2=r2, hd=hd, tg=tg: e.scalar_tensor_tensor(
                                out=qT[:, tg * 4:(tg + 1) * 4, hd, :], in0=PS[pb].rearrange("p (a b) -> p a b", a=4),
                                scalar=gq[:, 0:1], in1=rsb[r2].rearrange("p (a b) -> p a b", a=4), op0=ALU.mult,
                                op1=ALU.mult), reads=[f'ps{pb}', f'rsb{r2}', 'gq'], writes=[f'qT{tg}'])
                        else:
                            S.op('dve', lambda e, pb=pb, r2=r2, tg=tg: e.scalar_tensor_tensor(
                                out=kT[:, tg * 512:(tg + 1) * 512], in0=PS[pb], scalar=gk[:, 0:1], in1=rsb[r2],
                                op0=ALU.mult, op1=ALU.mult), reads=[f'ps{pb}', f'rsb{r2}', 'gk'], writes=['kT'])
                    pbv = 4 + tg % 2

                    def mmv(e, tg=tg, pbv=pbv):
                        for tt in range(4):
                            t = tg * 4 + tt
                            for c in range(16):
                                ins = e.matmul(PS[pbv][:, tt * 128:(tt + 1) * 128], lhsT=HT[:, c, t * 128:(t + 1) * 128],
                                               rhs=wv[:, c, :], start=(c == 0), stop=(c == 15))
                        return ins
                    S.op('pe', mmv, reads=['wv', f'HT{tg}'], writes=[f'ps{pbv}'])
                    copy_op(evac_engine(), Vg[:, tg * 4:(tg + 1) * 4, :], PS[pbv].rearrange("p (a b) -> p a b", a=4),
                            reads=[f'ps{pbv}'], writes=['Vg'])
                def stage_a(qb, kv=kv):
                    nonlocal kq
                    kbs = [kb for kb in (qb - 1, qb, qb + 1) if 0 <= kb < 16]
                    pts = []
                    for kb in kbs:
                        j = kb - qb + 1
                        pb = kq % 4
                        r6 = kq % 6
                        kq += 1

                        def mms(e, kb=kb, qb=qb, j=j, pb=pb, kv=kv):
                            e.matmul(PS[pb], lhsT=kT[:, kb * 128:(kb + 1) * 128],
                                     rhs=qT[:, qb, :, :].rearrange("p a b -> p (a b)"), start=True, stop=False)
                            return e.matmul(PS[pb], lhsT=idb,
                                            rhs=biasT[:, j, kv * 4:(kv + 1) * 4, :].rearrange("p a b -> p (a b)"),
                                            start=False, stop=True)
                        S.op('pe', mms, reads=['kT', f'qT{qb // 4}', 'biasT', 'idb'], writes=[f'ps{pb}'])
                        S.op('act', lambda e, pb=pb, r6=r6: e.activation(out=PT[r6], in_=PS[pb], func=AF.Exp,
                                                                         scale=SCALE),
                             reads=[f'ps{pb}'], writes=[f'PT{r6}'])
                        pts.append((kb, r6))
                    return pts

                def stage_b(qb, pts, kv=kv):
                    b2 = qb % 2
                    ppv, pdn = 4 + b2, 6 + b2

                    def mmpv(e, pts=pts, ppv=ppv, pdn=pdn):
                        for i, (kb, r6) in enumerate(pts):
                            e.matmul(PS[ppv], lhsT=Vg[:, kb, :], rhs=PT[r6], start=(i == 0), stop=(i == len(pts) - 1))
                        for i, (kb, r6) in enumerate(pts):
                            ins = e.matmul(PS[pdn], lhsT=onesb, rhs=PT[r6], start=(i == 0), stop=(i == len(pts) - 1))
                        return ins
                    S.op('pe', mmpv, reads=['Vg', 'onesb'] + [f'PT{r6}' for _, r6 in pts],
                         writes=[f'ps{ppv}', f'ps{pdn}'])
                    S.op('dve', lambda e, pdn=pdn, b2=b2, kv=kv: e.tensor_tensor(
                        out=denb[b2], in0=PS[pdn], in1=sinkexp[:, kv * 4:(kv + 1) * 4, :].rearrange("p a b -> p (a b)"),
                        op=ALU.add), reads=[f'ps{pdn}', 'sinkexp'], writes=[f'denb{b2}'])
                    S.op('dve', lambda e, b2=b2: e.reciprocal(out=rdb[b2], in_=denb[b2]), reads=[f'denb{b2}'],
                         writes=[f'rdb{b2}'])
                    S.op('dve', lambda e, ppv=ppv, b2=b2, qb=qb: e.tensor_tensor(
                        out=aT[:, :, qb * 128:(qb + 1) * 128], in0=PS[ppv].rearrange("p (a b) -> p a b", a=4),
                        in1=rdb[b2].rearrange("p (a b) -> p a b", a=4), op=ALU.mult),
                        reads=[f'ps{ppv}', f'rdb{b2}'], writes=['aT'])

                nxt = stage_a(0)
                for qb in range(16):
                    cur = nxt
                    if qb + 1 < 16:
                        nxt = stage_a(qb + 1)
                    stage_b(qb, cur)
                S.dma('sp', msc[kv * 4:(kv + 1) * 4].rearrange("g p t -> p g t"), aT, reads=['aT'], writes=['msc'])
            S.barrier()
            es3.close()
            proj_residual(HT, w_o, src, xa)

    def ffn_phase(src, dst, gain, moe):
        F = FFE if moe else FF0
        NJ = F // 128
        CP = 7 if moe else 11
        NP = NJ // CP
        with ExitStack() as es:
            st = alloc_norm_state(es)
            hTg = es.enter_context(sbt("hTg", [128, 16, 512], BF16)).ap()
            heT = es.enter_context(sbt("heT", [128, NJ, 512], BF16)).ap()
            gus = [es.enter_context(sbt(f"gus{i}", [128, 16, 256], BF16)).ap() for i in range(4)]
            wdp = [es.enter_context(sbt(f"wdp{i}", [128, CP, 512], BF16)).ap() for i in range(3)]
            sgb = [es.enter_context(sbt(f"sgb{i}", [128, 512], F32)).ap() for i in range(2)]
            if moe:
                acc = [es.enter_context(sbt(f"acc{i}", [128, D], F32)).ap() for i in range(4)]
                wrs = es.enter_context(sbt("wrs", [128, 16, NE], BF16)).ap()
                lg = [es.enter_context(sbt(f"lg{i}", [128, 8], F32)).ap() for i in range(4)]
                mx = [es.enter_context(sbt(f"mx{i}", [128, 8], F32)).ap() for i in range(4)]
                gw = [es.enter_context(sbt(f"gw{i}", [128, 4], F32)).ap() for i in range(4)]
                gt = [es.enter_context(sbt(f"gt{i}", [128, 8], F32)).ap() for i in range(4)]
                gt2 = [es.enter_context(sbt(f"gtb{i}", [128, 8], F32)).ap() for i in range(4)]
                S.dma('pool', wrs, w_r.rearrange("(c p) n -> p c n", p=128), writes=['wrs'])
            else:
                xp = [es.enter_context(sbt(f"fx{i}", [128, 512], F32)).ap() for i in range(3)]
                op_ = [es.enter_context(sbt(f"fo{i}", [128, 512], F32)).ap() for i in range(3)]
            S.dma('sp', st['gb'], gain.broadcast_to([128, D]), writes=['gb'])
            kk = {'gu': 0, 'wd': 0, 'ps': 0, 'x': 0}
            def prep_norm(tg_, tt_):
                t_ = tg_ * 4 + tt_
                norm_tile(st, src[t_ * 128:(t_ + 1) * 128, :], st['gb'], t_ % 2)

            def prep_tr(tg_, tt_):
                transpose_tile(st, (tg_ * 4 + tt_) % 2, hTg, tt_ * 128, 'hTg', banks=(0, 1))

            pipelined = not moe
            for tg in range(4):
                for tt in range(4):
                    t = tg * 4 + tt
                    b = t % 2
                    if not pipelined:
                        prep_norm(tg, tt)
                        prep_tr(tg, tt)
                    elif tg == 0:
                        if tt == 0:
                            prep_norm(0, 0)
                        if tt + 1 < 4:
                            prep_norm(0, tt + 1)
                        prep_tr(0, tt)
                    if moe:
                        S.op('pool', lambda e, tt=tt, b=b: e.tensor_copy(out=acc[tt], in_=st['xt'][b]),
                             reads=[f'xt{b}'], writes=[f'acc{tt}'])

                        def mml(e, tt=tt):
                            for c in range(16):
                                ins = e.matmul(PS[2][:, 0:NE], lhsT=hTg[:, c, tt * 128:(tt + 1) * 128], rhs=wrs[:, c, :],
                                               start=(c == 0), stop=(c == 15))
                            return ins
                        S.op('pe', mml, reads=['hTg', 'wrs'], writes=['ps2'])
                        S.op('dve', lambda e, tt=tt: e.tensor_copy(out=lg[tt], in_=PS[2][:, 0:NE]), reads=['ps2'],
                             writes=[f'lg{tt}'])
                        S.op('dve', lambda e, tt=tt: e.max(out=mx[tt], in_=lg[tt]), reads=[f'lg{tt}'],
                             writes=[f'mx{tt}'])
                        S.op('dve', lambda e, tt=tt: e.tensor_tensor(out=gw[tt][:, 0:1], in0=mx[tt][:, 0:1],
                                                                     in1=mx[tt][:, 1:2], op=ALU.subtract),
                             reads=[f'mx{tt}'], writes=[f'gwa{tt}'])
                        S.op('act', lambda e, tt=tt: e.activation(out=gw[tt][:, 1:2], in_=gw[tt][:, 0:1],
                                                                  func=AF.Sigmoid),
                             reads=[f'gwa{tt}'], writes=[f'gwb{tt}'])
                        S.op('act', lambda e, tt=tt: e.activation(out=gw[tt][:, 2:3], in_=gw[tt][:, 0:1],
                                                                  func=AF.Sigmoid, scale=-1.0),
                             reads=[f'gwa{tt}'], writes=[f'gwc{tt}'])
                        S.op('dve', lambda e, tt=tt: e.tensor_scalar(out=gt[tt], in0=lg[tt], scalar1=mx[tt][:, 0:1],
                                                                     scalar2=gw[tt][:, 1:2], op0=ALU.is_equal,
                                                                     op1=ALU.mult),
                             reads=[f'lg{tt}', f'mx{tt}', f'gwb{tt}'], writes=[f'gt{tt}'])
                        S.op('dve', lambda e, tt=tt: e.tensor_scalar(out=gt2[tt], in0=lg[tt], scalar1=mx[tt][:, 1:2],
                                                                     scalar2=gw[tt][:, 2:3], op0=ALU.is_equal,
                                                                     op1=ALU.mult),
                             reads=[f'lg{tt}', f'mx{tt}', f'gwc{tt}'], writes=[f'gtb{tt}'])
                        S.op('dve', lambda e, tt=tt: e.tensor_tensor(out=gt[tt], in0=gt[tt], in1=gt2[tt], op=ALU.add),
                             reads=[f'gt{tt}', f'gtb{tt}'], writes=[f'gt{tt}'])
                for ex in range(NE if moe else 1):
                    if moe:
                        Wg = we_g[ex].rearrange("(c p) n -> p c n", p=128)
                        Wu = we_u[ex].rearrange("(c p) n -> p c n", p=128)
                        Wd = we_d[ex].rearrange("(c p) n -> p c n", p=128)
                    else:
                        Wg = w_gu[:, 0:F].rearrange("(c p) n -> p c n", p=128)
                        Wu = w_gu[:, F:2 * F].rearrange("(c p) n -> p c n", p=128)
                        Wd = w_dn.rearrange("(c p) n -> p c n", p=128)
                    for jp in range(NJ // 2):
                        sg_, su_ = kk['gu'] % 4, (kk['gu'] + 1) % 4
                        kk['gu'] += 2
                        S.dma('pool', gus[sg_], Wg[:, :, jp * 256:(jp + 1) * 256], writes=[f'gus{sg_}'])
                        S.dma('pool', gus[su_], Wu[:, :, jp * 256:(jp + 1) * 256], writes=[f'gus{su_}'])
                        for jj in range(2):
                            j = jp * 2 + jj
                            pg = kk['ps'] % 2
                            pu = 2 + kk['ps'] % 2
                            kk['ps'] += 1

                            def mmg(e, sg_=sg_, jj=jj, pg=pg):
                                for c in range(16):
                                    ins = e.matmul(PS[pg], lhsT=gus[sg_][:, c, jj * 128:(jj + 1) * 128], rhs=hTg[:, c, :],
                                                   start=(c == 0), stop=(c == 15))
                                return ins

                            def mmu(e, su_=su_, jj=jj, pu=pu):
                                for c in range(16):
                                    ins = e.matmul(PS[pu], lhsT=gus[su_][:, c, jj * 128:(jj + 1) * 128], rhs=hTg[:, c, :],
                                                   start=(c == 0), stop=(c == 15))
                                return ins
                            S.op('pe', mmg, reads=[f'gus{sg_}', 'hTg'], writes=[f'ps{pg}'])
                            S.op('pe', mmu, reads=[f'gus{su_}', 'hTg'], writes=[f'ps{pu}'])
                            S.op('act', lambda e, pg=pg: e.activation(out=sgb[pg], in_=PS[pg], func=AF.Silu),
                                 reads=[f'ps{pg}'], writes=[f'sgb{pg}'])
                            S.op('dve', lambda e, pg=pg, pu=pu, j=j: e.tensor_tensor(out=heT[:, j, :], in0=sgb[pg],
                                                                                     in1=PS[pu], op=ALU.mult),
                                 reads=[f'sgb{pg}', f'ps{pu}'], writes=['heT'])
                    for n in range(4):
                        if pipelined and tg + 1 < 4:
                            prep_norm(tg + 1, n)
                        for p in range(NP):
                            wi = kk['wd'] % 3
                            kk['wd'] += 1
                            S.dma('pool', wdp[wi], Wd[:, p * CP:(p + 1) * CP, n * 512:(n + 1) * 512],
                                  writes=[f'wdp{wi}'])
                            for tt in range(4):
                                def mmd(e, wi=wi, tt=tt, p=p):
                                    for cc in range(CP):
                                        ins = e.matmul(PS[4 + tt], lhsT=heT[:, p * CP + cc, tt * 128:(tt + 1) * 128],
                                                       rhs=wdp[wi][:, cc, :], start=(p == 0 and cc == 0),
                                                       stop=(p == NP - 1 and cc == CP - 1))
                                    return ins
                                S.op('pe', mmd, reads=['heT', f'wdp{wi}'], writes=[f'ps{4 + tt}'])
                        for tt in range(4):
                            t = tg * 4 + tt
                            if moe:
                                S.op('dve', lambda e, tt=tt, n=n, ex=ex: e.scalar_tensor_tensor(
                                    out=acc[tt][:, n * 512:(n + 1) * 512], in0=PS[4 + tt], scalar=gt[tt][:, ex:ex + 1],
                                    in1=acc[tt][:, n * 512:(n + 1) * 512], op0=ALU.mult, op1=ALU.add),
                                    reads=[f'ps{4 + tt}', f'gt{tt}', f'acc{tt}'], writes=[f'acc{tt}'])
                            else:
                                r3 = kk['x'] % 3
                                kk['x'] += 1
                                S.dma('sp', xp[r3], src[t * 128:(t + 1) * 128, n * 512:(n + 1) * 512],
                                      writes=[f'fx{r3}'])
                                S.op('dve', lambda e, tt=tt, r3=r3: e.tensor_tensor(out=op_[r3], in0=PS[4 + tt],
                                                                                    in1=xp[r3], op=ALU.add),
                                     reads=[f'ps{4 + tt}', f'fx{r3}'], writes=[f'fo{r3}'])
                                S.dma('sp', dst[t * 128:(t + 1) * 128, n * 512:(n + 1) * 512], op_[r3],
                                      reads=[f'fo{r3}'], writes=['dst'])
                        if pipelined and tg + 1 < 4:
                            prep_tr(tg + 1, n)
                if moe:
                    for tt in range(4):
                        t = tg * 4 + tt
                        S.dma('sp', dst[t * 128:(t + 1) * 128, :], acc[tt], reads=[f'acc{tt}'], writes=['dst'])
            S.barrier()


    def moe_routed_phase(src, dst, gain):
        I32 = mybir.dt.int32
        WG, NCB = 896, 8
        NJ = FFE // 128
        with ExitStack() as es:
            S1i = es.enter_context(sbt("S1i", [128, 16], I32)).ap()
            S2i = es.enter_context(sbt("S2i", [128, 16], I32)).ap()
            W1 = es.enter_context(sbt("W1", [128, 16], F32)).ap()
            W2 = es.enter_context(sbt("W2", [128, 16], F32)).ap()
            IG = es.enter_context(sbt("IG", [128, 16], I32)).ap()
            ID = es.enter_context(sbt("ID", [128, 16], I32)).ap()
            with ExitStack() as es1:
                st = alloc_norm_state(es1)
                HTok = es1.enter_context(sbt("HTok", [128, 16, D], BF16)).ap()
                hT1 = [es1.enter_context(sbt(f"hT1{i}", [128, 16, 128], BF16)).ap() for i in range(2)]
                wrs = es1.enter_context(sbt("wrs", [128, 16, NE], BF16)).ap()
                trib = es1.enter_context(sbt("trib", [128, 128], BF16)).ap()
                pcol = es1.enter_context(sbt("pcol", [128, 1], F32)).ap()
                lg = es1.enter_context(sbt("lg", [128, 16, 8], F32)).ap()
                mx = es1.enter_context(sbt("mx", [128, 16, 8], F32)).ap()
                eq1 = es1.enter_context(sbt("eq1", [128, 16, 8], F32)).ap()
                eq2 = es1.enter_context(sbt("eq2", [128, 16, 8], F32)).ap()
                Mb = es1.enter_context(sbt("Mb", [128, 16, 8], BF16)).ap()
                CS = es1.enter_context(sbt("CS", [128, 16, 8], F32)).ap()
                TT = es1.enter_context(sbt("TT", [128, 16, 8], F32)).ap()
                OFF = es1.enter_context(sbt("OFF", [128, 16, 8], F32)).ap()
                Gp = es1.enter_context(sbt("Gp", [128, 16, 8], F32)).ap()
                tmp3 = es1.enter_context(sbt("tmp3", [128, 16, 8], F32)).ap()
                sm = es1.enter_context(sbt("sm", [128, 96], F32)).ap()
                S1f = es1.enter_context(sbt("S1f", [128, 16], F32)).ap()
                S2f = es1.enter_context(sbt("S2f", [128, 16], F32)).ap()
                IGf = es1.enter_context(sbt("IGf", [128, 16], F32)).ap()
                IDf = es1.enter_context(sbt("IDf", [128, 16], F32)).ap()
                S.dma('sp', st['gb'], gain.broadcast_to([128, D]), writes=['gb'])
                S.dma('pool', wrs, w_r.rearrange("(c p) n -> p c n", p=128), writes=['wrs'])
                S.dma('pool', trib, tri_d, writes=['trib'])
                S.dma('sp', pcol, pcol_d, writes=['pcol'])
                norm_tile(st, src[0:128, :], st['gb'], 0, dst=HTok[:, 0, :], dtag='HTok0')
                for t in range(NT):
                    b = t % 2
                    if t + 1 < NT:
                        norm_tile(st, src[(t + 1) * 128:(t + 2) * 128, :], st['gb'], (t + 1) % 2, dst=HTok[:, t + 1, :],
                                  dtag=f'HTok{t + 1}')
                    transpose_tile(st, b, hT1[b], 0, f'hT1{b}', banks=(6, 7), src=HTok[:, t, :], stag=f'HTok{t}')
                    pl = 4 + b

                    def mml(e, b=b, pl=pl):
                        for c in range(16):
                            ins = e.matmul(PS[pl][:, 0:NE], lhsT=hT1[b][:, c, :], rhs=wrs[:, c, :], start=(c == 0),
                                           stop=(c == 15))
                        return ins
                    S.op('pe', mml, reads=[f'hT1{b}', 'wrs'], writes=[f'ps{pl}'])
                    S.op('dve', lambda e, t=t, pl=pl: e.tensor_copy(out=lg[:, t, :], in_=PS[pl][:, 0:NE]),
                         reads=[f'ps{pl}'], writes=['lg'])
                    S.op('dve', lambda e, t=t: e.max(out=mx[:, t, :], in_=lg[:, t, :]), reads=['lg'], writes=['mx'])
                S.op('dve', lambda e: e.tensor_tensor(out=sm[:, 0:16], in0=mx[:, :, 0], in1=mx[:, :, 1],
                                                      op=ALU.subtract), reads=['mx'], writes=['sm_d'])
                S.op('act', lambda e: e.activation(out=W1, in_=sm[:, 0:16], func=AF.Sigmoid), reads=['sm_d'],
                     writes=['W1'])
                S.op('act', lambda e: e.activation(out=W2, in_=sm[:, 0:16], func=AF.Sigmoid, scale=-1.0),
                     reads=['sm_d'], writes=['W2'])
                S.op('dve', lambda e: e.tensor_tensor(out=eq1, in0=lg, in1=mx[:, :, 0:1].broadcast_to([128, 16, 8]),
                                                      op=ALU.is_equal), reads=['lg', 'mx'], writes=['eq1'])
                S.op('dve', lambda e: e.tensor_tensor(out=eq2, in0=lg, in1=mx[:, :, 1:2].broadcast_to([128, 16, 8]),
                                                      op=ALU.is_equal), reads=['lg', 'mx'], writes=['eq2'])
                S.op('dve', lambda e: e.tensor_tensor(out=Mb, in0=eq1, in1=eq2, op=ALU.add), reads=['eq1', 'eq2'],
                     writes=['Mb'])
                Mb2 = Mb.rearrange("p t e -> p (t e)")
                S.op('pe', lambda e: e.matmul(PS[0][:, 0:128], lhsT=trib, rhs=Mb2, start=True, stop=True),
                     reads=['Mb', 'trib'], writes=['ps0'])
                S.op('pe', lambda e: e.matmul(PS[1][:, 0:128], lhsT=onesb, rhs=Mb2, start=True, stop=True),
                     reads=['Mb', 'onesb'], writes=['ps1'])
                S.op('dve', lambda e: e.tensor_copy(out=CS.rearrange("p t e -> p (t e)"), in_=PS[0][:, 0:128]),
                     reads=['ps0'], writes=['CS'])
                S.op('dve', lambda e: e.tensor_copy(out=TT.rearrange("p t e -> p (t e)"), in_=PS[1][:, 0:128]),
                     reads=['ps1'], writes=['TT'])
                S.op('dve', lambda e: e.memset(OFF[:, 0, :], 0.0), writes=['OFF'])
                for t in range(1, NT):
                    S.op('dve', lambda e, t=t: e.tensor_tensor(out=OFF[:, t, :], in0=OFF[:, t - 1, :],
                                                               in1=TT[:, t - 1, :], op=ALU.add),
                         reads=['OFF', 'TT'], writes=['OFF'])
                cnt = sm[:, 16:24]
                nsl = sm[:, 24:32]
                cum = sm[:, 32:40]
                bas = sm[:, 40:48]
                t8 = sm[:, 48:56]
                ek = sm[:, 56:72]
                S.op('dve', lambda e: e.tensor_tensor(out=cnt, in0=OFF[:, 15, :], in1=TT[:, 15, :], op=ALU.add),
                     reads=['OFF', 'TT'], writes=['cnt'])
                S.op('dve', lambda e: e.tensor_scalar(out=nsl, in0=cnt, scalar1=0.0, scalar2=None, op0=ALU.is_gt),
                     reads=['cnt'], writes=['nsl'])
                for kk_ in (1, 2, 3):
                    S.op('dve', lambda e, kk_=kk_: e.tensor_scalar(out=t8, in0=cnt, scalar1=512.0 * kk_, scalar2=None,
                                                                   op0=ALU.is_gt), reads=['cnt'], writes=['t8'])
                    S.op('dve', lambda e: e.tensor_tensor(out=nsl, in0=nsl, in1=t8, op=ALU.add), reads=['nsl', 't8'],
                         writes=['nsl'])
                S.op('dve', lambda e: e.tensor_copy(out=cum[:, 0:1], in_=nsl[:, 0:1]), reads=['nsl'], writes=['cum'])
                for ee in range(1, NE):
                    S.op('dve', lambda e, ee=ee: e.tensor_tensor(out=cum[:, ee:ee + 1], in0=cum[:, ee - 1:ee],
                                                                 in1=nsl[:, ee:ee + 1], op=ALU.add),
                         reads=['cum', 'nsl'], writes=['cum'])
                S.op('dve', lambda e: e.tensor_tensor(out=bas, in0=cum, in1=nsl, op=ALU.subtract), reads=['cum', 'nsl'],
                     writes=['bas'])
                S.op('dve', lambda e: e.tensor_scalar(out=bas, in0=bas, scalar1=512.0, scalar2=-1.0, op0=ALU.mult,
                                                      op1=ALU.add), reads=['bas'], writes=['bas'])
                S.op('dve', lambda e: e.tensor_tensor(out=Gp, in0=CS, in1=OFF, op=ALU.add), reads=['CS', 'OFF'],
                     writes=['Gp'])
                S.op('dve', lambda e: e.tensor_tensor(out=Gp, in0=Gp, in1=bas.unsqueeze(1).broadcast_to([128, 16, 8]),
                                                      op=ALU.add), reads=['Gp', 'bas'], writes=['Gp'])
                S.op('dve', lambda e: e.tensor_tensor(out=tmp3, in0=Gp, in1=eq1, op=ALU.mult), reads=['Gp', 'eq1'],
                     writes=['tmp3'])
                S.op('dve', lambda e: e.tensor_reduce(out=S1f, in_=tmp3, axis=AX.X, op=ALU.add), reads=['tmp3'],
                     writes=['S1f'])
                S.op('dve', lambda e: e.tensor_tensor(out=tmp3, in0=Gp, in1=eq2, op=ALU.mult), reads=['Gp', 'eq2'],
                     writes=['tmp3'])
                S.op('dve', lambda e: e.tensor_reduce(out=S2f, in_=tmp3, axis=AX.X, op=ALU.add), reads=['tmp3'],
                     writes=['S2f'])
                S.op('dve', lambda e: e.tensor_copy(out=S1i, in_=S1f), reads=['S1f'], writes=['S1i'])
                S.op('dve', lambda e: e.tensor_copy(out=S2i, in_=S2f), reads=['S2f'], writes=['S2i'])
                for k_ in range(NSS):
                    S.op('dve', lambda e, k_=k_: e.tensor_scalar(out=t8, in0=cum, scalar1=float(k_), scalar2=None,
                                                                 op0=ALU.is_le), reads=['cum'], writes=['t8'])
                    S.op('dve', lambda e, k_=k_: e.tensor_reduce(out=ek[:, k_:k_ + 1], in_=t8, axis=AX.X, op=ALU.add),
                         reads=['t8'], writes=['ek'])
                S.op('dve', lambda e: e.tensor_scalar(out=ek, in0=ek, scalar1=float(NE - 1), scalar2=None, op0=ALU.min),
                     reads=['ek'], writes=['ek'])
                S.op('dve', lambda e: e.tensor_scalar(out=IGf, in0=ek, scalar1=float(D * NCB), scalar2=None,
                                                      op0=ALU.mult), reads=['ek'], writes=['IGf'])
                S.op('dve', lambda e: e.scalar_tensor_tensor(out=IGf, in0=pcol.broadcast_to([128, 16]),
                                                             scalar=float(NCB), in1=IGf, op0=ALU.mult, op1=ALU.add),
                     reads=['IGf', 'pcol'], writes=['IGf'])
                S.op('dve', lambda e: e.tensor_scalar(out=IDf, in0=ek, scalar1=float(FFE * 2), scalar2=None,
                                                      op0=ALU.mult), reads=['ek'], writes=['IDf'])
                S.op('dve', lambda e: e.scalar_tensor_tensor(out=IDf, in0=pcol.broadcast_to([128, 16]), scalar=2.0,
                                                             in1=IDf, op0=ALU.mult, op1=ALU.add),
                     reads=['IDf', 'pcol'], writes=['IDf'])
                S.op('dve', lambda e: e.tensor_copy(out=IG, in_=IGf), reads=['IGf'], writes=['IG'])
                S.op('dve', lambda e: e.tensor_copy(out=ID, in_=IDf), reads=['IDf'], writes=['ID'])
                for t in range(NT):
                    S.idma(hs_d, HTok[:, t, :], S1i[:, t:t + 1], scatter=True, reads=[f'HTok{t}', 'S1i', 'hs_d'],
                           writes=[f'hs_s{t}a'])
                    S.idma(hs_d, HTok[:, t, :], S2i[:, t:t + 1], scatter=True, reads=[f'HTok{t}', 'S2i', 'hs_d'],
                           writes=[f'hs_s{t}b'])
                S.barrier()
            with ExitStack() as es2:
                hTs = es2.enter_context(sbt("hTs", [128, 16, 512], BF16)).ap()
                hst = [es2.enter_context(sbt(f"hst{i}", [128, D], BF16)).ap() for i in range(2)]
                heT = es2.enter_context(sbt("heT", [128, NJ, 512], BF16)).ap()
                slab = [es2.enter_context(sbt(f"slab{i}", [128, 16, WG], BF16)).ap() for i in range(3)]
                wdp = [es2.enter_context(sbt(f"wdq{i}", [128, 1024], BF16)).ap() for i in range(5)]
                sgs = es2.enter_context(sbt("sgs", [128, 7, 512], BF16)).ap()
                ost = [es2.enter_context(sbt(f"ost{i}", [128, 512], BF16)).ap() for i in range(8)]
                Wg2 = we_g.rearrange("e d (cb w) -> (e d cb) w", w=WG)
                Wu2 = we_u.rearrange("e d (cb w) -> (e d cb) w", w=WG)
                Wd2 = we_d.rearrange("e f (nh w) -> (e f nh) w", w=1024)
                loads = []
                for k_ in range(NSS):
                    for cb in range(NCB):
                        for which in (0, 1):
                            loads.append(('s', k_, cb, which))
                    for nh in range(2):
                        for j in range(NJ):
                            loads.append(('d', k_, nh, j))
                pos = {L: i for i, L in enumerate(loads)}
                stt = {'emitted': 0, 'ns': 0, 'nd': 0}
                slot_of = {}

                def emit_load(L):
                    if L[0] == 's':
                        _, k_, cb, which = L
                        r = stt['ns'] % 3
                        stt['ns'] += 1
                        slot_of[L] = r
                        Wsrc = Wg2 if which == 0 else Wu2
                        for c in range(16):
                            S.idma(slab[r][:, c, :], Wsrc, IG[:, k_:k_ + 1], eoff=(c * 128 * NCB + cb) * WG,
                                   reads=['IG'], writes=[f'slab{r}_{c}'])
                    else:
                        _, k_, nh, j = L
                        r = stt['nd'] % 5
                        stt['nd'] += 1
                        slot_of[L] = r
                        S.idma(wdp[r], Wd2, ID[:, k_:k_ + 1], eoff=(j * 128 * 2 + nh) * 1024, reads=['ID'],
                               writes=[f'wdq{r}'])

                ring_cap = {'s': 3, 'd': 5}
                done = {'s': 0, 'd': 0}

                def ensure(L, ahead):
                    upto = min(len(loads), pos[L] + 1 + ahead)
                    while stt['emitted'] < upto:
                        nxt = loads[stt['emitted']]
                        ty = nxt[0]
                        if stt['n' + ty] - done[ty] >= ring_cap[ty]:
                            break
                        emit_load(nxt)
                        stt['emitted'] += 1

                ev = 0

                def load_hst(k_, tt):
                    b = tt % 2
                    S.dma('sp', hst[b], hs_d[(k_ * 4 + tt) * 128:(k_ * 4 + tt + 1) * 128, :], writes=[f'hst{b}'])

                load_hst(0, 0)
                load_hst(0, 1)
                for k_ in range(NSS):
                    for tt in range(4):
                        b = tt % 2
                        if tt >= 2:
                            load_hst(k_, tt)
                        transpose_tile(None, b, hTs, tt * 128, 'hTs', banks=(4, 5), src=hst[b], stag=f'hst{b}')
                    if k_ + 1 < NSS:
                        load_hst(k_ + 1, 0)
                        load_hst(k_ + 1, 1)
                    for cb in range(NCB):
                        for which in (0, 1):
                            L = ('s', k_, cb, which)
                            ensure(L, 2)
                            r = slot_of[L]
                            for jj in range(7):
                                j = cb * 7 + jj
                                pb = (0 if which == 0 else 2) + jj % 2

                                def mm(e, r=r, jj=jj, pb=pb):
                                    for c in range(16):
                                        ins = e.matmul(PS[pb], lhsT=slab[r][:, c, jj * 128:(jj + 1) * 128],
                                                       rhs=hTs[:, c, :], start=(c == 0), stop=(c == 15))
                                    return ins
                                S.op('pe', mm, reads=[f'slab{r}_{c}' for c in range(16)] + ['hTs'], writes=[f'ps{pb}'])
                                if which == 0:
                                    S.op('act', lambda e, pb=pb, jj=jj: e.activation(out=sgs[:, jj, :], in_=PS[pb],
                                                                                     func=AF.Silu),
                                         reads=[f'ps{pb}'], writes=[f'sgs{jj}'])
                                else:
                                    S.op('dve', lambda e, pb=pb, jj=jj, j=j: e.tensor_tensor(
                                        out=heT[:, j, :], in0=sgs[:, jj, :], in1=PS[pb], op=ALU.mult),
                                        reads=[f'sgs{jj}', f'ps{pb}'], writes=['heT'])
                            done['s'] += 1
                    for nh in range(2):
                        for j in range(NJ):
                            L = ('d', k_, nh, j)
                            ensure(L, 4)
                            r = slot_of[L]

                            def mmd(e, r=r, j=j):
                                for tt in range(4):
                                    for n2 in range(2):
                                        ins = e.matmul(PS[tt * 2 + n2], lhsT=heT[:, j, tt * 128:(tt + 1) * 128],
                                                       rhs=wdp[r][:, n2 * 512:(n2 + 1) * 512], start=(j == 0),
                                                       stop=(j == NJ - 1))
                                return ins
                            S.op('pe', mmd, reads=['heT', f'wdq{r}'], writes=[f'ps{i}' for i in range(8)])
                            done['d'] += 1
                        for tt in range(4):
                            for n2 in range(2):
                                o4 = ev % 8
                                ev += 1
                                pb = tt * 2 + n2
                                copy_op(evac_engine(), ost[o4], PS[pb], reads=[f'ps{pb}'], writes=[f'ost{o4}'])
                                r0 = k_ * 512 + tt * 128
                                c0 = nh * 1024 + n2 * 512
                                S.dma('sp', outs_d[r0:r0 + 128, c0:c0 + 512], ost[o4], reads=[f'ost{o4}'],
                                      writes=['outs_d'])
                S.barrier()
            with ExitStack() as es3:
                xt = [es3.enter_context(sbt(f"cx{i}", [128, D], F32)).ap() for i in range(2)]
                g1 = [es3.enter_context(sbt(f"cg1{i}", [128, D], BF16)).ap() for i in range(2)]
                g2 = [es3.enter_context(sbt(f"cg2{i}", [128, D], BF16)).ap() for i in range(2)]
                ac = [es3.enter_context(sbt(f"cac{i}", [128, D], F32)).ap() for i in range(2)]
                for t in range(NT):
                    b = t % 2
                    S.dma('sp', xt[b], src[t * 128:(t + 1) * 128, :], writes=[f'cx{b}'])
                    S.idma(g1[b], outs_d, S1i[:, t:t + 1], reads=['S1i'], writes=[f'cg1{b}'])
                    S.idma(g2[b], outs_d, S2i[:, t:t + 1], reads=['S2i'], writes=[f'cg2{b}'])
                    S.op('dve', lambda e, b=b, t=t: e.scalar_tensor_tensor(out=ac[b], in0=g1[b], scalar=W1[:, t:t + 1],
                                                                           in1=xt[b], op0=ALU.mult, op1=ALU.add),
                         reads=[f'cg1{b}', f'cx{b}', 'W1'], writes=[f'cac{b}'])
                    S.op('dve', lambda e, b=b, t=t: e.scalar_tensor_tensor(out=ac[b], in0=g2[b], scalar=W2[:, t:t + 1],
                                                                           in1=ac[b], op0=ALU.mult, op1=ALU.add),
                         reads=[f'cg2{b}', f'cac{b}', 'W2'], writes=[f'cac{b}'])
                    S.dma('sp', dst[t * 128:(t + 1) * 128, :], ac[b], reads=[f'cac{b}'], writes=['dst'])
                S.barrier()

    def gmlp_phase(src, dst):
        with ExitStack() as es:
            HT = es.enter_context(sbt("HT", [128, 16, T], BF16)).ap()
            with ExitStack() as es2:
                st = alloc_norm_state(es2)
                norm_transpose_all(st, src, g_l1_mix, HT)
                S.barrier()
            es3 = ExitStack()
            vsb = es3.enter_context(sbt("vsb", [128, 16, D], BF16)).ap()
            wb = [es3.enter_context(sbt(f"gw{i}", [128, 16, 512], BF16)).ap() for i in range(2)]
            vgb = es3.enter_context(sbt("vgb", [128, D], F32)).ap()
            wsT = es3.enter_context(sbt("wsT", [128, 16, 128], BF16)).ap()
            bsb = es3.enter_context(sbt("bsb", [128, 16, 128], F32)).ap()
            tmp = [es3.enter_context(sbt(f"gtmp{i}", [128, 512], F32)).ap() for i in range(2)]
            junk = es3.enter_context(sbt("gjunk", [128, 512], BF16)).ap()
            ssv = es3.enter_context(sbt("ssv", [128, 64], F32)).ap()
            ss1 = es3.enter_context(sbt("ss1", [128, 48], F32)).ap()
            es4 = ExitStack()
            wsn = es4.enter_context(sbt("wsn", [128, 16, 128], BF16)).ap()

            S.dma('sp', vgb, g_v.broadcast_to([128, D]), writes=['vgb'])
            S.dma('sp', bsb.rearrange("p g t -> p (g t)"), b_s.broadcast_to([128, 16 * 128]), writes=['bsb'])
            S.dma('pool', wsn, w_s.rearrange("g t s -> t g s"), writes=['wsn'])
            for hf in range(2):
                def trw(e, hf=hf):
                    for c in range(8):
                        ins = e.transpose(out=PSB[hf][:, c * 128:(c + 1) * 128], in_=wsn[:, hf * 8 + c, :], identity=idb)
                    return ins
                S.op('pe', trw, reads=['wsn', 'idb'], writes=[f'ps{hf}'])
                copy_op('dve', wsT[:, hf * 8:(hf + 1) * 8, :], PSB[hf].rearrange("p (c t) -> p c t", c=8),
                        reads=[f'ps{hf}'], writes=['wsT'])
            S.barrier()
            es4.close()
            ugb = [es3.enter_context(sbt(f"ugb{i}", [128, 512], F32)).ap() for i in range(2)]
            vpb = [es3.enter_context(sbt(f"vpb{i}", [128, 512], F32)).ap() for i in range(2)]
            yTj = [es3.enter_context(sbt(f"yTj{i}", [128, T], BF16)).ap() for i in range(2)]
            Wv_ = w_in.rearrange("(c p) n -> p c n", p=128)
            k = 0
            for n in range(4):
                wbn = n % 2
                S.dma('pool', wb[wbn], Wv_[:, :, D + n * 512:D + (n + 1) * 512], writes=[f'gw{wbn}'])
                for t in range(NT):
                    pb = 2 + k % 4
                    r3 = k % 2
                    k += 1

                    def mm(e, t=t, wbn=wbn, pb=pb):
                        for c in range(16):
                            ins = e.matmul(PS[pb], lhsT=HT[:, c, t * 128:(t + 1) * 128], rhs=wb[wbn][:, c, :],
                                           start=(c == 0), stop=(c == 15))
                        return ins
                    S.op('pe', mm, reads=[f'HT{t // 4}', f'gw{wbn}'], writes=[f'ps{pb}'])
                    S.op('act', lambda e, pb=pb, r3=r3: e.activation(out=tmp[r3], in_=PS[pb], func=AF.Gelu_apprx_tanh),
                         reads=[f'ps{pb}'], writes=[f'gtmp{r3}'])
                    S.op('act', lambda e, r3=r3, t=t, n=n: e.activation(out=junk, in_=tmp[r3], func=AF.Square,
                                                                        accum_out=ssv[:, t * 4 + n:t * 4 + n + 1]),
                         reads=[f'gtmp{r3}'], writes=['gjunk', 'ssv'])
                    S.op('dve', lambda e, r3=r3, t=t, n=n: e.tensor_copy(out=vsb[:, t, n * 512:(n + 1) * 512],
                                                                         in_=tmp[r3]),
                         reads=[f'gtmp{r3}'], writes=[f'vsb{t}'])
            S.op('dve', lambda e: e.tensor_reduce(out=ss1[:, 0:16], in_=ssv.rearrange("p (t n) -> p t n", n=4),
                                                  axis=AX.X, op=ALU.add), reads=['ssv'], writes=['ss1a'])
            S.op('act', lambda e: e.activation(out=ss1[:, 16:32], in_=ss1[:, 0:16], func=AF.Sqrt, scale=1.0 / D,
                                               bias=EPS), reads=['ss1a'], writes=['ss1b'])
            S.op('dve', lambda e: e.reciprocal(out=ss1[:, 32:48], in_=ss1[:, 16:32]), reads=['ss1b'], writes=['ss1c'])
            for t in range(NT):
                S.op('dve', lambda e, t=t: e.scalar_tensor_tensor(out=vsb[:, t, :], in0=vsb[:, t, :],
                                                                  scalar=ss1[:, 32 + t:33 + t], in1=vgb, op0=ALU.mult,
                                                                  op1=ALU.mult),
                     reads=[f'vsb{t}', 'ss1c', 'vgb'], writes=[f'vsb{t}'])
            for jp in range(8):
                wbn = jp % 2
                S.dma('pool', wb[wbn][:, :, 0:256], Wv_[:, :, jp * 256:(jp + 1) * 256], writes=[f'gw{wbn}'])
                for jj in range(2):
                    j = jp * 2 + jj
                    yb = j % 2
                    for tg in range(4):
                        pu = k % 2
                        pv = 2 + k % 2
                        k += 1

                        def mmu(e, wbn=wbn, jj=jj, tg=tg, pu=pu):
                            for c in range(16):
                                ins = e.matmul(PS[pu], lhsT=wb[wbn][:, c, jj * 128:(jj + 1) * 128],
                                               rhs=HT[:, c, tg * 512:(tg + 1) * 512], start=(c == 0), stop=(c == 15))
                            return ins
                        S.op('pe', mmu, reads=[f'gw{wbn}', f'HT{tg}'], writes=[f'ps{pu}'])

                        def mmv(e, j=j, tg=tg, pv=pv):
                            for tt in range(4):
                                ins = e.matmul(PS[pv][:, tt * 128:(tt + 1) * 128],
                                               lhsT=vsb[:, tg * 4 + tt, j * 128:(j + 1) * 128], rhs=wsT[:, j, :],
                                               start=True, stop=True)
                            return ins
                        S.op('pe', mmv, reads=[f'vsb{tg * 4 + tt}' for tt in range(4)] + ['wsT'], writes=[f'ps{pv}'])
                        S.op('act', lambda e, pu=pu: e.activation(out=ugb[pu], in_=PS[pu], func=AF.Gelu_apprx_tanh),
                             reads=[f'ps{pu}'], writes=[f'ugb{pu}'])
                        S.op('dve', lambda e, pv=pv, pu=pu, j=j: e.tensor_tensor(
                            out=vpb[pu].rearrange("p (a b) -> p a b", a=4),
                            in0=PS[pv].rearrange("p (a b) -> p a b", a=4),
                            in1=bsb[:, j:j + 1, :].broadcast_to([128, 4, 128]), op=ALU.add),
                            reads=[f'ps{pv}', 'bsb'], writes=[f'vpb{pu}'])
                        S.op('dve', lambda e, pu=pu, yb=yb, tg=tg: e.tensor_tensor(
                            out=yTj[yb][:, tg * 512:(tg + 1) * 512], in0=vpb[pu], in1=ugb[pu], op=ALU.mult),
                            reads=[f'vpb{pu}', f'ugb{pu}'], writes=[f'yTj{yb}'])
                    S.dma('sp', msc[j], yTj[yb], reads=[f'yTj{yb}'], writes=['msc'])
            S.barrier()
            es3.close()
            proj_residual(HT, w_out, src, dst)

    S.barrier()
    if stage >= 1:
        attention_phase(x_in, hs_d if stage >= 4 else None)
    last = xa
    if stage >= 2:
        ffn_phase(xa, xb, g_l0_ffn, moe=False)
        last = xb
    if stage >= 3:
        gmlp_phase(xb, xc)
        last = xc
    if stage >= 4:
        moe_routed_phase(xc, y_out, g_l1_ffn)
    else:
        with ExitStack() as es:
            cp = [es.enter_context(sbt(f"cp{i}", [128, D], F32)).ap() for i in range(2)]
            for t in range(NT):
                S.dma('sp', cp[t % 2], last[t * 128:(t + 1) * 128, :], reads=['dst'], writes=[f'cp{t % 2}'])
                S.dma('sp', y_out[t * 128:(t + 1) * 128, :], cp[t % 2], reads=[f'cp{t % 2}'], writes=['y'])
    S.barrier()
    return nc


def make_consts():
    ident = np.eye(128, dtype=np.float32)
    slopes = 2.0 ** (-8.0 * np.arange(1, 17, dtype=np.float64) / 16)
    s = np.arange(128)[:, None]
    q = np.arange(128)[None, :]
    bias = np.zeros((128, 3, 16, 128), np.float32)
    for j in range(3):
        delta = q - s + (1 - j) * 128
        dist = np.abs(delta)
        valid = dist <= 128
        for h in range(16):
            b = np.where(valid, -slopes[h] * dist, -1e30) / SCALE
            bias[:, j, h, :] = b.astype(np.float32)
    return ident, bias.reshape(128, 3 * 16 * 128)


_NC_CACHE = {}


def kernel(**inputs):
    stage = int(inputs.pop('_stage', 99))
    ncores = int(inputs.pop('_ncores', NCORES))
    if stage not in _NC_CACHE:
        _NC_CACHE[stage] = build_program(stage)
    nc = _NC_CACHE[stage]
    ident, bias = make_consts()
    f = lambda a: np.ascontiguousarray(np.asarray(a, dtype=np.float32))
    shared = {
        'l0_mix_norm': f(inputs['l0_mix_norm']).reshape(1, D),
        'l0_w_qkv': f(inputs['l0_w_qkv']),
        'l0_q_norm': f(inputs['l0_q_norm']).reshape(128, 1),
        'l0_k_norm': f(inputs['l0_k_norm']).reshape(128, 1),
        'l0_sink': f(inputs['l0_sink']).reshape(1, 16),
        'l0_w_o': f(inputs['l0_w_o']),
        'l0_ffn_norm': f(inputs['l0_ffn_norm']).reshape(1, D),
        'l0_w_gate_up': f(inputs['l0_w_gate_up']),
        'l0_w_down': f(inputs['l0_w_down']),
        'l1_mix_norm': f(inputs['l1_mix_norm']).reshape(1, D),
        'l1_w_in': f(inputs['l1_w_in']),
        'l1_v_norm': f(inputs['l1_v_norm']).reshape(1, D),
        'l1_w_s': f(inputs['l1_w_s']),
        'l1_b_s': f(inputs['l1_b_s']).reshape(1, 16 * 128),
        'l1_w_out': f(inputs['l1_w_out']),
        'l1_ffn_norm': f(inputs['l1_ffn_norm']).reshape(1, D),
        'l1_w_router': f(inputs['l1_w_router']) if stage >= 4 else None,
        'l1_we_gate': f(inputs['l1_we_gate']) if stage >= 4 else None,
        'l1_we_up': f(inputs['l1_we_up']) if stage >= 4 else None,
        'l1_we_down': f(inputs['l1_we_down']) if stage >= 4 else None,
        'c_ident': ident,
        'c_bias': bias,
        'c_tri': np.triu(np.ones((128, 128), np.float32)),
        'c_pcol': np.arange(128, dtype=np.float32).reshape(128, 1),
    }
    if stage < 4:
        for kname in ('l1_w_router', 'l1_we_gate', 'l1_we_up', 'l1_we_down', 'c_tri', 'c_pcol'):
            shared.pop(kname)
    x = f(inputs['x'])
    in_maps = []
    for c in range(ncores):
        m = dict(shared)
        m['x'] = x[c]
        in_maps.append(m)
    res = run_bass_kernel_spmd(nc, in_maps, core_ids=list(range(ncores)))
    return np.stack([np.asarray(r['y'], dtype=np.float32) for r in res.results], axis=0)
```
